# Optimizing a Trainium2 kernel written in Bass

```python
import jax, jax.numpy as jnp
from jax import lax
import numpy as np

D_MODEL = 4096
BATCH = 4
SEQ = 2048
DEPTH = 1

NORM_EPS = 1e-6
N_MOD = 6
MIX_WIDTH = D_MODEL
GLA_WIDTH = MIX_WIDTH // 2
GLA_DV = 128
GLA_HEADS = GLA_WIDTH // GLA_DV
GLA_DK = GLA_DV // 2
GLA_KEY_WIDTH = GLA_HEADS * GLA_DK
GLA_GATE_RANK = 16
GLA_GATE_TEMP = 16.0
GLA_CHUNK = 64
SWA_HEAD_DIM = 64
SWA_Q_HEADS = (MIX_WIDTH - GLA_WIDTH) // SWA_HEAD_DIM
SWA_KV_HEADS = SWA_Q_HEADS // 8
SWA_Q_WIDTH = SWA_Q_HEADS * SWA_HEAD_DIM
SWA_KV_WIDTH = SWA_KV_HEADS * SWA_HEAD_DIM
SWA_WINDOW = 128
SWA_BLOCK = 128
ROPE_THETA = 10000.0
IN_SIZES = (GLA_KEY_WIDTH, GLA_KEY_WIDTH, GLA_WIDTH, GLA_WIDTH, GLA_GATE_RANK,
            SWA_Q_WIDTH, SWA_KV_WIDTH, SWA_KV_WIDTH)
IN_WIDTH = sum(IN_SIZES)
IN_OFFSETS = tuple(sum(IN_SIZES[:i + 1]) for i in range(len(IN_SIZES) - 1))
PEER_HEADS = 8
PEER_N_KEYS = 128
PEER_N_EXPERTS = PEER_N_KEYS * PEER_N_KEYS
PEER_QUERY_DIM = 256
PEER_HALF_DIM = PEER_QUERY_DIM // 2
PEER_TOPK = 16
PEER_TOKEN_BLOCK = 64

kernel_name = 'hymba_gla_swa_peer_adaln'


def _rms(x, gain=None):
    xf = x.astype(jnp.float32)
    y = xf * lax.rsqrt(jnp.mean(xf * xf, axis=-1, keepdims=True) + NORM_EPS)
    if gain is not None:
        y = y * gain.astype(jnp.float32)
    return y.astype(x.dtype)


def _modulate(x, shift, scale):
    return _rms(x) * (1 + scale) + shift


def _rope(x, pos):
    half = x.shape[-1] // 2
    inv = ROPE_THETA ** (-jnp.arange(half, dtype=jnp.float32) / half)
    ang = pos.astype(jnp.float32)[:, None] * inv[None, :]
    cos = jnp.cos(ang)[None, :, None, :]
    sin = jnp.sin(ang)[None, :, None, :]
    xf = x.astype(jnp.float32)
    x1, x2 = xf[..., :half], xf[..., half:]
    return jnp.concatenate([x1 * cos - x2 * sin, x2 * cos + x1 * sin], axis=-1).astype(x.dtype)


def _gla(q, k, v, log_a):
    B, S, H, dk = q.shape
    dv = v.shape[-1]
    C = GLA_CHUNK
    n = S // C

    def to_chunks(t):
        return t.reshape(B, n, C, H, t.shape[-1]).transpose(1, 0, 3, 2, 4)

    qc, kc, vc, ac = (to_chunks(t) for t in (q * (dk ** -0.5), k, v, log_a))
    causal = jnp.tril(jnp.ones((C, C), dtype=bool))

    def step(state, inp):
        qi, ki, vi, ai = inp
        b = jnp.cumsum(ai.astype(jnp.float32), axis=2)
        diff = b[:, :, :, None, :] - b[:, :, None, :, :]
        decay = jnp.exp(jnp.where(causal[:, :, None], diff, -jnp.inf))
        attn = jnp.einsum('bhtd,bhsd,bhtsd->bhts', qi, ki, decay)
        o = (jnp.einsum('bhts,bhsv->bhtv', attn, vi)
             + jnp.einsum('bhtd,bhdv->bhtv', qi * jnp.exp(b), state))
        b_last = b[:, :, -1:, :]
        state = (jnp.exp(b_last[:, :, 0, :])[..., None] * state
                 + jnp.einsum('bhsd,bhsv->bhdv', ki * jnp.exp(b_last - b), vi))
        return state, o

    state0 = jnp.zeros((B, H, dk, dv), jnp.float32)
    _, o = lax.scan(step, state0, (qc, kc, vc, ac))
    return o.transpose(1, 0, 3, 2, 4).reshape(B, S, H, dv)


def _swa(q, k, v, sinks):
    B, S, Hq, dh = q.shape
    Hkv = k.shape[2]
    G = Hq // Hkv
    W = SWA_BLOCK
    n = S // W
    qb = q.reshape(B, n, W, Hkv, G, dh)

    def with_prev(t):
        tb = t.reshape(B, n, W, Hkv, dh)
        prev = jnp.pad(tb[:, :-1], ((0, 0), (1, 0), (0, 0), (0, 0), (0, 0)))
        return jnp.concatenate([prev, tb], axis=2)

    kb, vb = with_prev(k), with_prev(v)
    s = jnp.einsum('bnqkgd,bnskd->bnkgqs', qb, kb).astype(jnp.float32) * (dh ** -0.5)
    qpos = jnp.arange(n)[:, None] * W + jnp.arange(W)[None, :]
    kpos = jnp.arange(n)[:, None] * W - W + jnp.arange(2 * W)[None, :]
    rel = qpos[:, :, None] - kpos[:, None, :]
    mask = (rel >= 0) & (rel < SWA_WINDOW) & (kpos[:, None, :] >= 0)
    s = jnp.where(mask[None, :, None, None], s, -jnp.inf)
    sink = sinks.astype(jnp.float32).reshape(Hkv, G)[None, None, :, :, None, None]
    m = jnp.maximum(jnp.max(s, axis=-1, keepdims=True), sink)
    p = jnp.exp(s - m)
    p = p / (jnp.sum(p, axis=-1, keepdims=True) + jnp.exp(sink - m))
    o = jnp.einsum('bnkgqs,bnskd->bnqkgd', p.astype(v.dtype), vb)
    return o.reshape(B, S, Hq, dh)


def _peer(y, w_q, keys_1, keys_2, down, up):
    B, S, D = y.shape
    H, K = PEER_HEADS, PEER_TOPK
    q = (y @ w_q).reshape(B, S, H, 2, PEER_HALF_DIM)
    s1 = jnp.einsum('bshd,kd->bshk', q[..., 0, :], keys_1).astype(jnp.float32)
    s2 = jnp.einsum('bshd,kd->bshk', q[..., 1, :], keys_2).astype(jnp.float32)
    v1, i1 = lax.top_k(s1, K)
    v2, i2 = lax.top_k(s2, K)
    cand = (v1[..., :, None] + v2[..., None, :]).reshape(B, S, H, K * K)
    cidx = (i1[..., :, None] * PEER_N_KEYS + i2[..., None, :]).reshape(B, S, H, K * K)
    top_s, pos = lax.top_k(cand, K)
    idx = jnp.take_along_axis(cidx, pos, axis=-1)
    gate = jax.nn.softmax(top_s, axis=-1)
    T = B * S
    nb = T // PEER_TOKEN_BLOCK
    yt = y.reshape(nb, PEER_TOKEN_BLOCK, D)
    it = idx.reshape(nb, PEER_TOKEN_BLOCK, H * K)
    gt = gate.reshape(nb, PEER_TOKEN_BLOCK, H * K)

    def block(args):
        yb, ib, gb = args
        u = jnp.take(down, ib, axis=0)
        vv = jnp.take(up, ib, axis=0)
        h = jax.nn.gelu(jnp.einsum('td,tkd->tk', yb, u).astype(jnp.float32), approximate=False) * gb
        return jnp.einsum('tk,tkd->td', h.astype(yb.dtype), vv)

    out = lax.map(block, (yt, it, gt))
    return out.reshape(B, S, D)


def setup_inputs(seed: int = 0) -> dict:
    key = jax.random.key(seed)
    ks = jax.random.split(key, 20)
    f32 = jnp.float32
    nrm = lambda k, shape, std: jax.random.normal(k, shape, f32) * std
    gain = lambda k, shape: 1.0 + 0.02 * jax.random.normal(k, shape, f32)
    L, D = DEPTH, D_MODEL
    return {
        'x': nrm(ks[0], (BATCH, SEQ, D), 1.0),
        'c': nrm(ks[1], (BATCH, D), 1.0),
        'w_ada': nrm(ks[2], (L, D, N_MOD * D), 0.005),
        'b_ada': nrm(ks[3], (L, N_MOD * D), 0.02),
        'w_in': nrm(ks[4], (L, D, IN_WIDTH), D ** -0.5),
        'w_gla_gate_up': nrm(ks[5], (L, GLA_GATE_RANK, GLA_KEY_WIDTH), GLA_GATE_RANK ** -0.5),
        'b_gla_gate': nrm(ks[6], (L, GLA_KEY_WIDTH), 0.1),
        'gla_out_norm': gain(ks[7], (L, GLA_DV)),
        'swa_q_norm': gain(ks[8], (L, SWA_HEAD_DIM)),
        'swa_k_norm': gain(ks[9], (L, SWA_HEAD_DIM)),
        'swa_sinks': nrm(ks[10], (L, SWA_Q_HEADS), 1.0),
        'swa_out_norm': gain(ks[11], (L, SWA_HEAD_DIM)),
        'w_out': nrm(ks[12], (L, MIX_WIDTH, D), MIX_WIDTH ** -0.5),
        'w_peer_q': nrm(ks[13], (L, D, PEER_HEADS * PEER_QUERY_DIM), D ** -0.5),
        'peer_sub_keys_1': nrm(ks[14], (L, PEER_N_KEYS, PEER_HALF_DIM), PEER_HALF_DIM ** -0.5),
        'peer_sub_keys_2': nrm(ks[15], (L, PEER_N_KEYS, PEER_HALF_DIM), PEER_HALF_DIM ** -0.5),
        'peer_expert_down': nrm(ks[16], (L, PEER_N_EXPERTS, D), D ** -0.5),
        'peer_expert_up': nrm(ks[17], (L, PEER_N_EXPERTS, D), 1.0),
    }


def reference(x, c, w_ada, b_ada, w_in, w_gla_gate_up, b_gla_gate, gla_out_norm,
              swa_q_norm, swa_k_norm, swa_sinks, swa_out_norm, w_out, w_peer_q,
              peer_sub_keys_1, peer_sub_keys_2, peer_expert_down, peer_expert_up):
    B, S, D = x.shape
    pos = jnp.arange(S)
    c_act = jax.nn.silu(c)
    for l in range(DEPTH):
        mod = (c_act @ w_ada[l] + b_ada[l])[:, None, :]
        sh1, sc1, g1, sh2, sc2, g2 = jnp.split(mod, N_MOD, axis=-1)

        h = _modulate(x, sh1, sc1)
        proj = h @ w_in[l]
        gq, gk, gv, gg, ga, sq, sk, sv = jnp.split(proj, IN_OFFSETS, axis=-1)

        log_a = jax.nn.log_sigmoid((ga @ w_gla_gate_up[l] + b_gla_gate[l]).astype(jnp.float32)) / GLA_GATE_TEMP
        o_gla = _gla(gq.reshape(B, S, GLA_HEADS, GLA_DK), gk.reshape(B, S, GLA_HEADS, GLA_DK),
                     gv.reshape(B, S, GLA_HEADS, GLA_DV), log_a.reshape(B, S, GLA_HEADS, GLA_DK))
        o_gla = _rms(o_gla, gla_out_norm[l]) * jax.nn.silu(gg.reshape(B, S, GLA_HEADS, GLA_DV))
        o_gla = o_gla.reshape(B, S, GLA_WIDTH)

        qh = _rope(_rms(sq.reshape(B, S, SWA_Q_HEADS, SWA_HEAD_DIM), swa_q_norm[l]), pos)
        kh = _rope(_rms(sk.reshape(B, S, SWA_KV_HEADS, SWA_HEAD_DIM), swa_k_norm[l]), pos)
        vh = sv.reshape(B, S, SWA_KV_HEADS, SWA_HEAD_DIM)
        o_swa = _rms(_swa(qh, kh, vh, swa_sinks[l]), swa_out_norm[l]).reshape(B, S, SWA_Q_WIDTH)

        mix = jnp.concatenate([o_gla.astype(x.dtype), o_swa.astype(x.dtype)], axis=-1) @ w_out[l]
        x = x + g1 * mix

        y = _modulate(x, sh2, sc2)
        x = x + g2 * _peer(y, w_peer_q[l], peer_sub_keys_1[l], peer_sub_keys_2[l],
                           peer_expert_down[l], peer_expert_up[l])
    return x
```

```python
import numpy as np
from contextlib import ExitStack
import concourse.bass as bass
import concourse.mybir as mybir
from concourse.bass_utils import run_bass_kernel_spmd

F32 = mybir.dt.float32
BF16 = mybir.dt.bfloat16
U32 = mybir.dt.uint32
AF = mybir.ActivationFunctionType
ALU = mybir.AluOpType
AX = mybir.AxisListType

D = 4096
NCH = 32
T = 1024
EPS = 1e-6
N_CORES = 8


class StopBuild(Exception):
    def __init__(self, nc):
        self.nc = nc


class Buf:
    __slots__ = ("name", "w", "r", "excl")

    def __init__(self, name, excl=False):
        self.name = name
        self.w = None
        self.r = []
        self.excl = excl


class _Rec:
    def __init__(self):
        self.calls = []

    def __getattr__(self, name):
        def f(*a, **k):
            self.calls.append((name, a, k))
            return self
        return f


class Prog:
    ENG = ("tensor", "vector", "scalar", "gpsimd", "sync")

    def __init__(self, nc, es):
        self.nc = nc
        self.es = es
        self.ops = {e: [] for e in self.ENG}
        self.cnt = {}
        self.waited = {e: {} for e in self.ENG}
        self.sems = {}
        for e in self.ENG:
            self.sem(("eng", e))

    def sem(self, key):
        if key not in self.sems:
            name = "s_" + "_".join(str(k) for k in key) if isinstance(key, tuple) else "s_" + str(key)
            self.sems[key] = self.es.enter_context(self.nc.semaphore(name))
            self.cnt[key] = 0
        return key

    def op(self, eng, fn, reads=(), writes=(), dma=None):
        deps = {}

        def add(ev, is_raw):
            if ev is None:
                return
            k, v = ev
            if dma is None and k == ("eng", eng):
                if eng == "tensor":
                    return
            if deps.get(k, 0) < v:
                deps[k] = v

        for b in reads:
            add(b.w, True)
            if b.excl:
                for ev in b.r:
                    add(ev, False)
        for b in writes:
            add(b.w, False)
            for ev in b.r:
                add(ev, False)
        wt = self.waited[eng]
        waits = []
        for k, v in deps.items():
            if wt.get(k, 0) < v:
                wt[k] = v
                waits.append((k, v))
        if dma is None:
            key = ("eng", eng)
            self.cnt[key] += 1
            inc = 1
        else:
            key = self.sem(dma)
            self.cnt[key] += 16
            inc = 16
        ev = (key, self.cnt[key])
        for b in reads:
            b.r.append(ev)
            if len(b.r) > 64:
                m = {}
                for k, v in b.r:
                    if m.get(k, 0) < v:
                        m[k] = v
                b.r = list(m.items())
        for b in writes:
            b.w = ev
            b.r = []
        calls = None
        if fn is not None:
            rec = _Rec()
            fn(rec)
            calls = rec.calls
        self.ops[eng].append((waits, calls, key, inc))
        return ev

    def emit(self):
        nc = self.nc
        with nc.Block() as block:
            for e in self.ENG:
                ops = self.ops[e]
                if not ops:
                    continue

                def body(engine, ops=ops):
                    for waits, fn, key, inc in ops:
                        for k, v in waits:
                            engine.wait_ge(self.sems[k], v)
                        if fn is None:
                            ins = engine.nop()
                        else:
                            for name, a, kw in fn:
                                ins = getattr(engine, name)(*a, **kw)
                        ins.then_inc(self.sems[key], inc)

                getattr(block, e)(body)


def build_nc(stop=10**9, dbg=False):
    nc = bass.Bass("TRN2", target_bir_lowering=False)
    es = ExitStack()

    def din(name, shape, dt=F32):
        return nc.dram_tensor(name, list(shape), dt, kind="ExternalInput").ap()

    def dscr(name, shape, dt):
        return nc.dram_tensor(name, list(shape), dt).ap()

    xT = din("xT", [128, NCH, 2048])
    cT = din("cT", [128, NCH])
    w_ada = din("w_ada", [48, 128, NCH * 512])
    b_ada = din("b_ada", [1, 24576])
    w_in = din("w_in", [69, 128, 4096])
    wg = din("wg", [16, 1024])
    bg = din("bg", [1, 1024])
    gla_gain = din("gla_gain", [128, 1])
    qn = din("qn", [64])
    kn = din("kn", [64])
    on = din("on", [128, 1])
    sinks = din("sinks", [32])
    cs = din("cs", [128, 9, 2, 32])
    w_out = din("w_out", [32, 128, 4096])
    w_q = din("w_q", [16, 128, 4096])
    keysT = din("keysT", [128, 2, 128])
    down = din("down", [128, 128, 4096])
    up = din("up", [8, 16, 128, 4096])
    masks = din("masks", [128, 3, 128])
    ident = din("ident", [128, 128])
    iotar = din("iotar", [128, 128])
    flag = din("flag", [128, 1])
    outT = nc.dram_tensor("outT", [128, NCH, T], F32, kind="ExternalOutput").ap()
    dbgo = None
    if dbg:
        dbgo = nc.dram_tensor("dbgo", [128, 8192], F32, kind="ExternalOutput").ap()

    projo = dscr("projo", [69, 128, T], BF16)
    projp = dscr("projp", [69, 128, T], BF16)
    mixd = dscr("mixd", [32, 128, T], BF16)
    x1d = dscr("x1d", [32, 128, T], F32)
    Gd = dscr("Gd", [128, 128, T], BF16)
    hTd = dscr("hTd", [128, 128, T], BF16)

    with es:
        P = Prog(nc, es)

        def sb(name, shape, dt):
            return es.enter_context(nc.sbuf_tensor(name, list(shape), dt))

        H = sb("H", [128, 32768], BF16)
        Q = sb("Q", [128, 28672], BF16)
        X = sb("X", [128, 8192], F32)
        W = [sb(f"W{i}", [128, 4096], BF16) for i in range(2)]
        S = [sb(f"S{i}", [128, 1024], BF16) for i in range(3)]
        FSA = sb("FSA", [128, 2048], F32)
        FS = [FSA[:, 0:1024], FSA[:, 1024:2048]]
        bH, bQ, bX = Buf("H"), Buf("Q"), Buf("X")
        bW = [Buf(f"W{i}") for i in range(2)]
        bS = [Buf(f"S{i}") for i in range(3)]
        bFS = [Buf(f"FS{i}") for i in range(2)]
        pb = [es.enter_context(nc.psum_tensor(f"pb{i}", [128, 512], F32)) for i in range(8)]
        bP = [Buf(f"pb{i}", excl=True) for i in range(8)]

        modT = sb("modT", [128, 192], F32)
        bmod = Buf("modT")
        c_f = sb("c_f", [128, NCH], F32)
        c_bf = sb("c_bf", [128, NCH], BF16)
        bc = Buf("c")
        bbrow = [Buf("brow")] * 2
        bmrow = [Buf("mrow")] * 2
        one11 = sb("one11", [1, 1], F32)
        ones_bf = sb("ones_bf", [128, 128], BF16)
        ident_f = sb("ident_f", [128, 128], F32)
        ident_b = sb("ident_b", [128, 128], BF16)
        iota_f = sb("iota_f", [128, 128], F32)
        masks_f = sb("masks_f", [128, 3, 128], F32)
        ltri = sb("ltri", [128, 128], F32)
        flag_s = sb("flag_s", [128, 1], F32)
        gain_s = sb("gain_s", [128, 1], F32)
        on_s = sb("on_s", [128, 1], F32)
        FSb = FSA[:, :].bitcast(BF16)
        wg_b = FSb[0:16, 0:1024]
        bg_b = FSb[0:1, 1024:2048]
        bwg = Buf("wg")
        keys_b = sb("keys_b", [128, 2, 128], BF16)
        qn_b = sb("qn_b", [128, 64], F32)
        kn_b = sb("kn_b", [128, 64], F32)
        esink = sb("esink", [128, 32], F32)
        cs_s = sb("cs_s", [128, 9, 2, 32], F32)
        bconst = Buf("const")
        rstd = sb("rstd", [128, 1024], F32)
        brstd = Buf("rstd")
        bm_t = rstd[0:1, :]
        brow = [bm_t[0:1, 0:512], bm_t[0:1, 0:512]]
        mrow = [bm_t[0:1, 512:1024], bm_t[0:1, 512:1024]]
        state = sb("state", [128, 8, 128], F32)
        state_b = sb("state_b", [128, 8, 2, 128], BF16)
        bstate, bstate_b = Buf("state"), Buf("state_b")
        small = sb("small", [128, 256], F32)
        bsmall = Buf("small")

        def V(fn, r=(), w=()):
            return P.op("vector", fn, r, w)

        def A(fn, r=(), w=()):
            return P.op("scalar", fn, r, w)

        def PE(fn, r=(), w=()):
            return P.op("tensor", fn, r, w)

        def DS(fn, r=(), w=(), sem=None):
            return P.op("sync", fn, r, w, dma=sem)

        def DG(fn, r=(), w=(), sem=None):
            return P.op("gpsimd", fn, r, w, dma=sem)

        def DSsplit(dst_tiles, src_tiles, n, step, r, w, sem):
            for a in range(0, n, step):
                b_ = min(n, a + step)
                DS(lambda e, a=a, b_=b_: e.dma_start(out=dst_tiles(a, b_), in_=src_tiles(a, b_)), r=r, w=w, sem=sem)

        DS(lambda e: e.dma_start(out=ident_f[:, :], in_=ident), w=[bconst], sem="c0")
        DS(lambda e: e.dma_start(out=iota_f[:, :], in_=iotar), w=[bconst], sem="c0")
        DS(lambda e: e.dma_start(out=masks_f[:, :, :], in_=masks), w=[bconst], sem="c0")
        DS(lambda e: e.dma_start(out=flag_s[:, :], in_=flag), w=[bconst], sem="c0")
        DS(lambda e: e.dma_start(out=gain_s[:, :], in_=gla_gain), w=[bconst], sem="c0")
        DS(lambda e: e.dma_start(out=on_s[:, :], in_=on), w=[bconst], sem="c0")
        DS(lambda e: e.dma_start(out=cs_s[:, :, :, :], in_=cs), w=[bconst], sem="c0")
        DS(lambda e: e.dma_start(out=qn_b[:, :], in_=qn.partition_broadcast(128)), w=[bconst], sem="c0")
        DS(lambda e: e.dma_start(out=kn_b[:, :], in_=kn.partition_broadcast(128)), w=[bconst], sem="c0")
        DS(lambda e: e.dma_start(out=esink[:, :], in_=sinks.partition_broadcast(128)), w=[bconst], sem="c0")
        DS(lambda e: e.dma_start(out=c_f[:, :], in_=cT), w=[bc], sem="c2")
        DG(lambda e: e.dma_start(out=keys_b[:, :, :], in_=keysT), w=[bconst], sem="c1")
        V(lambda e: e.tensor_copy(out=ident_b[:, :], in_=ident_f[:, :]), r=[bconst], w=[bconst])
        V(lambda e: e.memset(ones_bf[:, :], 1.0), w=[bconst])
        V(lambda e: e.memset(one11[:, :], 1.0), w=[bconst])
        V(lambda e: e.tensor_scalar(out=ltri[:, :], in0=masks_f[:, 0, :], scalar1=-1.0 / 16.0, scalar2=None, op0=ALU.mult), r=[bconst], w=[bconst])
        A(lambda e: e.activation(out=esink[:, :], in_=esink[:, :], func=AF.Exp), r=[bconst], w=[bconst])
        A(lambda e: e.activation(out=c_f[:, :], in_=c_f[:, :], func=AF.Silu), r=[bc], w=[bc])
        V(lambda e: e.tensor_copy(out=c_bf[:, :], in_=c_f[:, :]), r=[bc], w=[bc])

        Hh = [H[:, 0:16384], H[:, 16384:32768]]
        bHh = [Buf("Hh0"), Buf("Hh1")]
        NADA = 48

        def ada_load(j, s, late=False):
            if late:
                DG(lambda e: e.dma_start(out=Hh[s], in_=w_ada[j], max_dma_last_dim=8192), r=[bH], w=[bHh[s]], sem=f"Hh{s}")
            else:
                DG(lambda e: e.dma_start(out=Hh[s], in_=w_ada[j], max_dma_last_dim=8192), w=[bHh[s], bH], sem=f"Hh{s}")

        def ada_comp(j, s, late=False):
            pa = pb[6] if late else pb[s]
            bpa = bP[6] if late else bP[s]
            DS(lambda e: e.dma_start(out=brow[0], in_=b_ada[:, j * 512:(j + 1) * 512]), w=[bbrow[0], brstd], sem="brow")

            def mm(e):
                for c in range(NCH):
                    ins = e.matmul(pa[0:1, :], lhsT=c_bf[:, c:c + 1], rhs=Hh[s][:, c * 512:(c + 1) * 512], start=(c == 0), stop=(c == NCH - 1))
                return ins
            PE(mm, r=[bHh[s], bc] + ([bH] if late else []), w=[bpa])
            V(lambda e: e.tensor_tensor(out=mrow[0], in0=pa[0:1, :], in1=brow[0], op=ALU.add), r=[bpa, bbrow[0]], w=[bmrow[0], brstd])

            def tr(e):
                for i in range(4):
                    ins = e.matmul(pb[7][:, i:i + 1], lhsT=bm_t[0:1, 512 + i * 128:512 + (i + 1) * 128], rhs=one11[0:1, 0:1], start=True, stop=True)
                return ins
            PE(tr, r=[bmrow[0], bconst, brstd], w=[bP[7]])
            V(lambda e: e.tensor_copy(out=modT[:, 4 * j:4 * j + 4], in_=pb[7][:, 0:4]), r=[bP[7]], w=[bmod])

        NADA0 = 16
        ada_load(0, 0)
        for j in range(NADA0):
            if j + 1 < NADA0:
                ada_load(j + 1, (j + 1) % 2)
            ada_comp(j, j % 2)
        V(lambda e: e.tensor_scalar(out=modT[:, 32:64], in0=modT[:, 32:64], scalar1=1.0, scalar2=None, op0=ALU.add), r=[bmod], w=[bmod])
        SH1, SC1, G1, SH2, SC2, G2 = 0, 32, 64, 96, 128, 160

        def dump(ap_sb, ncols, rbufs):
            DS(lambda e: e.dma_start(out=dbgo[:, 0:ncols], in_=ap_sb), r=rbufs, w=[bdbg], sem="dbg")

        bdbg = Buf("dbg")
        bout = Buf("out")

        def finish():
            P.op("sync", None, reads=[bdbg, bout])
            P.emit()

        if stop == 0:
            dump(modT[:, :], 192, [bmod])
            finish()
            return nc

        def rstd_from(ps_ap, out_ap, n, inv_n, rb, wb_):
            V(lambda e: e.tensor_scalar(out=out_ap, in0=ps_ap, scalar1=inv_n, scalar2=EPS, op0=ALU.mult, op1=ALU.add), r=rb, w=wb_)
            A(lambda e: e.activation(out=out_ap, in_=out_ap, func=AF.Sqrt), r=wb_, w=wb_)
            V(lambda e: e.reciprocal(out=out_ap, in_=out_ap), r=wb_, w=wb_)

        Hv = H[:, :].rearrange("p (c t) -> p c t", c=NCH)
        Xv = X[:, :].rearrange("p (c t) -> p c t", c=NCH)
        Qsq = Q[:, 0:8192].rearrange("p (c t) -> p c t", c=NCH)

        def make_hT(tok0):
            for blk in range(4):
                t0 = tok0 + blk * 256
                DS(lambda e: e.dma_start(out=Xv, in_=xT[:, :, t0:t0 + 256]), w=[bX], sem="X")
                A(lambda e: e.activation(out=Q[:, 0:8192], in_=X[:, :], func=AF.Square), r=[bX], w=[bQ])

                def mm(e):
                    for c in range(NCH):
                        ins = e.matmul(pb[4][:, 0:256], lhsT=ones_bf[:, :], rhs=Qsq[:, c, :], start=(c == 0), stop=(c == NCH - 1))
                    return ins
                PE(mm, r=[bQ, bconst], w=[bP[4]])
                rs = rstd[:, 0:256]
                rstd_from(pb[4][:, 0:256], rs, 256, 1.0 / D, [bP[4]], [brstd])
                V(lambda e: e.tensor_tensor(out=Xv, in0=Xv, in1=rs.unsqueeze(1).to_broadcast([128, NCH, 256]), op=ALU.mult), r=[bX, brstd], w=[bX])
                lo = blk * 256

                def modu(e):
                    for c in range(NCH):
                        ins = e.tensor_scalar(out=Hv[:, c, lo:lo + 256], in0=Xv[:, c, :], scalar1=modT[:, SC1 + c:SC1 + c + 1], scalar2=modT[:, SH1 + c:SH1 + c + 1], op0=ALU.mult, op1=ALU.add)
                    return ins
                V(modu, r=[bX, bmod], w=[bH])

        gemm_k = [0]

        def gemm(jobs, act_v, act_bufs, segs, consume, extra_load=None):
            n = len(jobs)

            def load(i):
                k = gemm_k[0] + i
                s = k % 2
                src = jobs[i][0]
                DG(lambda e: e.dma_start(out=W[s][:, :], in_=src, max_dma_last_dim=8192), w=[bW[s]], sem=f"W{s}")
                if extra_load is not None:
                    extra_load(i)

            def comp(i):
                k = gemm_k[0] + i
                s = k % 2
                banks = [2 * (k % 2) + q for q in range(len(segs))]

                def mm(e):
                    for c in range(NCH):
                        for q, (lo, nn) in enumerate(segs):
                            ins = e.matmul(pb[banks[q]][:, 0:nn], lhsT=W[s][:, c * 128:(c + 1) * 128], rhs=act_v[:, c, lo:lo + nn], start=(c == 0), stop=(c == NCH - 1))
                    return ins
                PE(mm, r=[bW[s]] + act_bufs, w=[bP[b_] for b_ in banks])
                consume(i, jobs[i][1], banks)

            depth = 1
            for i in range(n + depth):
                if i < n:
                    load(i)
                if i - depth >= 0:
                    comp(i - depth)
            gemm_k[0] += n

        evk = [0]

        def evac_to_dram(dst_fn, segs):
            def consume(i, tag, banks):
                k = evk[0]
                evk[0] += 1
                s = k % 3
                off = 0
                for q, (lo, nn) in enumerate(segs):
                    o0 = off
                    if (k + q) % 2 == 0:
                        A(lambda e: e.activation(out=S[s][:, o0:o0 + nn], in_=pb[banks[q]][:, 0:nn], func=AF.Copy), r=[bP[banks[q]]], w=[bS[s]])
                    else:
                        V(lambda e: e.tensor_copy(out=S[s][:, o0:o0 + nn], in_=pb[banks[q]][:, 0:nn]), r=[bP[banks[q]]], w=[bS[s]])
                    off += nn
                tot = off
                DS(lambda e: e.dma_start(out=dst_fn(tag), in_=S[s][:, 0:tot]), r=[bS[s]], w=[bprojd], sem=f"S{s}")
            return consume

        bprojd = Buf("projd")
        FULL = [(0, 512), (512, 512)]

        make_hT(0)
        pre_tiles = list(range(8, 32)) + [48]
        gemm([(w_in[j], j) for j in pre_tiles], Hv, [bH], FULL, evac_to_dram(lambda j: projp[j], FULL))
        seg128 = [(896, 128)]
        gemm([(w_in[j], j) for j in range(65, 69)], Hv, [bH], seg128, evac_to_dram(lambda j: projp[j][:, 0:128], seg128))
        make_hT(1024)
        gemm([(w_in[j], j) for j in range(69)], Hv, [bH], FULL, evac_to_dram(lambda j: projo[j], FULL))

        if stop == 1:
            DS(lambda e: e.dma_start(out=S[0][:, :], in_=projo[3]), r=[bprojd], w=[bS[0]], sem="S0")
            V(lambda e: e.tensor_copy(out=FS[0][:, :], in_=S[0][:, :]), r=[bS[0]], w=[bFS[0]])
            dump(FS[0][:, :], 1024, [bFS[0]])
            finish()
            return nc

        Qv = Q[:, :].rearrange("p (a t) -> p a t", t=T)
        gqT = Qv[:, 0:8, :]
        gkT = Qv[:, 8:16, :]
        gvT = Hv[:, 0:16, :]
        ggT = Hv[:, 16:32, :]
        ga_s = S[2][0:16, :]
        bga = bS[2]
        DG(lambda e: e.dma_start(out=wg_b, in_=wg), w=[bwg, bFS[0], bFS[1]], sem="c1")
        DG(lambda e: e.dma_start(out=bg_b, in_=bg), w=[bwg, bFS[0], bFS[1]], sem="c1")
        qo = 16384
        qtl = Q[:, qo:qo + 1024].rearrange("p (a t) -> p a t", a=8)
        ktz = Q[:, qo + 1024:qo + 3072].rearrange("p (a b t) -> p a b t", a=8, b=2)
        k2T = Q[:, qo + 3072:qo + 4096].rearrange("p (a t) -> p a t", a=8)
        k2s = Q[:, qo + 4096:qo + 5120]
        v_sb = Q[:, qo + 5120:qo + 7168]
        attn_m = Q[:, qo + 7168:qo + 9216].rearrange("p (a t) -> p a t", a=16)
        o_n = attn_m
        mixst = Q[:, qo + 9216:qo + 11264].rearrange("p (a t) -> p a t", a=16)
        bqt, bkt, bk2T, bk2s, bvs, battn, bmix = [Buf(n) for n in "qt kt k2T k2s vs attn mix".split()]
        bon = battn
        e1 = X[:, 0:1024]
        ebx = X[:, 1024:2048]
        enb = X[:, 2048:3072]
        ek2 = X[:, 3072:4096]
        sqx = X[:, 4096:6144]
        be1, beb, benb, bek2, bsq = [Buf(n) for n in "e1 eb enb ek2 sq".split()]
        blast = small[:, 0:8]
        eblast = small[:, 8:16]
        ss16 = small[:, 16:32]
        maskc = masks_f[:, 0, :]
        bmixd = Buf("mixd")

        V(lambda e: e.memset(state[:, :, :], 0.0), w=[bstate])
        V(lambda e: e.memset(state_b[:, :, :, :], 0.0), w=[bstate_b])
        V(lambda e: e.memset(ktz, 0.0), w=[bkt])

        class _Stop(Exception):
            pass
        ckn = [0]

        def ck(bufs):
            ckn[0] += 1
            if stop == 20 + ckn[0]:
                P.op("sync", None, reads=bufs)
                raise _Stop()

        def gla_pass(proj, own):
            DSsplit(lambda a, b: gkT[:, a:b, :], lambda a, b: proj[8 + a:8 + b].rearrange("a p t -> p a t"), 8, 4, [bprojd], [bQ], "Q")
            DSsplit(lambda a, b: gvT[:, a:b, :], lambda a, b: proj[16 + a:16 + b].rearrange("a p t -> p a t"), 16, 4, [bprojd], [bH], "H")
            DS(lambda e: e.dma_start(out=ga_s, in_=proj[48][0:16, :]), r=[bprojd], w=[bga], sem="ga")
            if own:
                DSsplit(lambda a, b: gqT[:, a:b, :], lambda a, b: proj[a:b].rearrange("a p t -> p a t"), 8, 4, [bprojd], [bQ], "Q")
                DSsplit(lambda a, b: ggT[:, a:b, :], lambda a, b: proj[32 + a:32 + b].rearrange("a p t -> p a t"), 16, 4, [bprojd], [bH], "H")
                A(lambda e: e.activation(out=H[:, 16384:32768], in_=H[:, 16384:32768], func=AF.Silu), r=[bH], w=[bH])
                V(lambda e: e.tensor_scalar(out=H[:, 16384:32768], in0=H[:, 16384:32768], scalar1=gain_s[:, 0:1], scalar2=None, op0=ALU.mult), r=[bH, bconst], w=[bH])
            ck([bQ, bH, bga])
            for c in range(8):
                cs_ = slice(c * 128, (c + 1) * 128)

                def zmm(e):
                    for nb in range(2):
                        e.matmul(pb[nb][:, :], lhsT=ga_s[0:16, cs_], rhs=wg_b[0:16, nb * 512:(nb + 1) * 512], start=True, stop=False)
                        ins = e.matmul(pb[nb][:, :], lhsT=ones_bf[0:1, 0:128], rhs=bg_b[0:1, nb * 512:(nb + 1) * 512], start=False, stop=True)
                    return ins
                PE(zmm, r=[bga, bconst, bwg], w=[bP[0], bP[1]])
                ck([bP[0], bP[1]])
                for nb in range(2):
                    A(lambda e, nb=nb: e.activation(out=e1[:, nb * 512:(nb + 1) * 512], in_=pb[nb][:, :], func=AF.Exp, scale=-1.0), r=[bP[nb]], w=[be1])
                A(lambda e: e.activation(out=e1, in_=e1, func=AF.Ln, bias=1.0), r=[be1], w=[be1])

                def bmm(e):
                    for p in range(8):
                        ins = e.matmul(pb[2 + p // 4][:, (p % 4) * 128:(p % 4 + 1) * 128], lhsT=e1[:, p * 128:(p + 1) * 128], rhs=ltri[:, :], start=True, stop=True)
                    return ins
                ck([be1])
                PE(bmm, r=[be1, bconst], w=[bP[2], bP[3]])
                ck([bP[2], bP[3]])
                for k in range(2):
                    V(lambda e, k=k: e.tensor_copy(out=blast[:, 4 * k:4 * k + 4], in_=pb[2 + k][:, 127::128]), r=[bP[2 + k]], w=[bsmall])
                for k in range(2):
                    if own:
                        A(lambda e, k=k: e.activation(out=ebx[:, k * 512:(k + 1) * 512], in_=pb[2 + k][:, :], func=AF.Exp), r=[bP[2 + k]], w=[beb])
                        A(lambda e, k=k: e.activation(out=enb[:, k * 512:(k + 1) * 512], in_=pb[2 + k][:, :], func=AF.Exp, scale=-1.0), r=[bP[2 + k]], w=[benb])
                for p in range(8):
                    A(lambda e, p=p: e.activation(out=ek2[:, p * 128:(p + 1) * 128], in_=pb[2 + p // 4][:, (p % 4) * 128:(p % 4 + 1) * 128], func=AF.Exp, scale=-1.0, bias=blast[:, p:p + 1]), r=[bP[2 + p // 4], bsmall], w=[bek2])
                A(lambda e: e.activation(out=eblast, in_=blast, func=AF.Exp), r=[bsmall], w=[bsmall])
                ck([bek2, bsmall, beb, benb])
                v3 = lambda ap: ap.rearrange("p (a t) -> p a t", a=8)
                if own:
                    V(lambda e: e.scalar_tensor_tensor(out=qtl, in0=gqT[:, :, cs_], scalar=0.125, in1=v3(ebx), op0=ALU.mult, op1=ALU.mult), r=[bQ, beb], w=[bqt])
                    for hh in range(2):
                        V(lambda e, hh=hh: e.tensor_tensor(out=ktz[hh * 64:hh * 64 + 64, :, hh, :], in0=gkT[hh * 64:hh * 64 + 64, :, cs_], in1=v3(enb)[hh * 64:hh * 64 + 64, :, :], op=ALU.mult), r=[bQ, benb], w=[bkt])
                V(lambda e: e.tensor_tensor(out=k2T, in0=gkT[:, :, cs_], in1=v3(ek2), op=ALU.mult), r=[bQ, bek2], w=[bk2T])
                pbK = pb[4][:, :].bitcast(BF16)
                pbV = [pb[5][:, :].bitcast(BF16), pb[6][:, :].bitcast(BF16)]

                def trk(e):
                    for p in range(8):
                        ins = e.transpose(out=pbK[:, p * 128:(p + 1) * 128], in_=k2T[:, p, :], identity=ident_b[:, :])
                    return ins
                ck([bk2T, bqt, bkt])
                PE(trk, r=[bk2T, bconst], w=[bP[4]])
                ck([bP[4]])

                def trv(e):
                    for h in range(16):
                        ins = e.transpose(out=pbV[h // 8][:, (h % 8) * 128:(h % 8 + 1) * 128], in_=gvT[:, h, cs_], identity=ident_b[:, :])
                    return ins
                PE(trv, r=[bH, bconst], w=[bP[5], bP[6]])
                ck([bP[5], bP[6]])
                A(lambda e: e.activation(out=k2s, in_=pbK, func=AF.Copy), r=[bP[4]], w=[bk2s])
                V(lambda e: e.tensor_copy(out=v_sb[:, 0:1024], in_=pbV[0]), r=[bP[5]], w=[bvs])
                A(lambda e: e.activation(out=v_sb[:, 1024:2048], in_=pbV[1], func=AF.Copy), r=[bP[6]], w=[bvs])
                if own:
                    def amm(e):
                        for h in range(16):
                            p, r0 = h // 2, (h % 2) * 64
                            ins = e.matmul(pb[2 * (h % 2) + p // 4][:, (p % 4) * 128:(p % 4 + 1) * 128], lhsT=ktz[:, p, h % 2, :], rhs=qtl[:, p, :], start=True, stop=True)
                        return ins
                    PE(amm, r=[bkt, bqt], w=[bP[0], bP[1], bP[2], bP[3]])
                    attn_v = attn_m.rearrange("p (a b) t -> p a b t", b=2)
                    for k in range(4):
                        V(lambda e, k=k: e.tensor_tensor(out=attn_v[:, 4 * (k % 2):4 * (k % 2) + 4, k // 2, :], in0=pb[k][:, :].rearrange("p (a t) -> p a t", a=4), in1=maskc.unsqueeze(1).to_broadcast([128, 4, 128]), op=ALU.mult), r=[bP[k], bconst], w=[battn])

                    def omm(e):
                        for h in range(16):
                            p, r0 = h // 2, (h % 2) * 64
                            o_ap = pb[4 + h // 4][:, (h % 4) * 128:(h % 4 + 1) * 128]
                            e.matmul(o_ap, lhsT=attn_m[:, h, :], rhs=v_sb[:, h * 128:(h + 1) * 128], start=True, stop=False)
                            ins = e.matmul(o_ap, lhsT=qtl[:, p, :], rhs=state_b[:, p, h % 2, :], start=False, stop=True)
                        return ins
                    PE(omm, r=[battn, bvs, bqt, bstate_b], w=[bP[4], bP[5], bP[6], bP[7]])

                def smm(e):
                    for p in range(8):
                        ins = e.matmul(pb[p // 2][:, (p % 2) * 256:(p % 2 + 1) * 256], lhsT=k2s[:, p * 128:(p + 1) * 128], rhs=v_sb[:, p * 256:(p + 1) * 256], start=True, stop=True)
                    return ins
                ck([bk2s, bvs])
                PE(smm, r=[bk2s, bvs], w=[bP[0], bP[1], bP[2], bP[3]])
                ck([bP[0], bP[1], bP[2], bP[3]])

                def supd(e):
                    for p in range(8):
                        for hh in range(2):
                            r0 = hh * 64
                            ins = e.scalar_tensor_tensor(out=state[r0:r0 + 64, p, :], in0=state[r0:r0 + 64, p, :], scalar=eblast[r0:r0 + 64, p:p + 1], in1=pb[p // 2][r0:r0 + 64, (p % 2) * 256 + hh * 128:(p % 2) * 256 + hh * 128 + 128], op0=ALU.mult, op1=ALU.add)
                    return ins
                V(supd, r=[bstate, bstate_b, bsmall, bP[0], bP[1], bP[2], bP[3]], w=[bstate])
                if (not own) and c == 7:
                    V(lambda e: e.tensor_scalar(out=state[:, :, :], in0=state[:, :, :], scalar1=flag_s[:, 0:1], scalar2=None, op0=ALU.mult), r=[bstate, bconst], w=[bstate])
                for hh in range(2):
                    V(lambda e, hh=hh: e.tensor_copy(out=state_b[hh * 64:hh * 64 + 64, :, hh, :], in_=state[hh * 64:hh * 64 + 64, :, :]), r=[bstate], w=[bstate_b])
                if own:
                    for k in range(4):
                        A(lambda e, k=k: e.activation(out=sqx[:, k * 512:(k + 1) * 512], in_=pb[4 + k][:, :], func=AF.Square), r=[bP[4 + k]], w=[bsq])
                    V(lambda e: e.tensor_reduce(out=ss16, in_=sqx.rearrange("p (a t) -> p a t", a=16), axis=AX.X, op=ALU.add), r=[bsq], w=[bsmall])
                    rstd_from(ss16, ss16, 16, 1.0 / 128, [bsmall], [bsmall])
                    for k in range(4):
                        V(lambda e, k=k: e.tensor_tensor(out=o_n[:, 4 * k:4 * k + 4, :], in0=pb[4 + k][:, :].rearrange("p (a t) -> p a t", a=4), in1=ss16[:, 4 * k:4 * k + 4].unsqueeze(2).to_broadcast([128, 4, 128]), op=ALU.mult), r=[bP[4 + k], bsmall], w=[bon])
                    pbT = [pb[4][:, :].bitcast(BF16), pb[5][:, :].bitcast(BF16)]

                    def tro(e):
                        for h in range(16):
                            ins = e.transpose(out=pbT[h // 8][:, (h % 8) * 128:(h % 8 + 1) * 128], in_=o_n[:, h, :], identity=ident_b[:, :])
                        return ins
                    PE(tro, r=[bon, bconst], w=[bP[4], bP[5]])
                    for k in range(2):
                        V(lambda e, k=k: e.tensor_tensor(out=mixst[:, 8 * k:8 * k + 8, :], in0=pbT[k].rearrange("p (a t) -> p a t", a=8), in1=ggT[:, 8 * k:8 * k + 8, cs_], op=ALU.mult), r=[bP[4 + k], bH], w=[bmix])
                    DSsplit(lambda a, b: mixd[a:b, :, cs_].rearrange("a p t -> p a t"), lambda a, b: mixst[:, a:b, :], 16, 4, [bmix], [bmixd], "mix")

        try:
            gla_pass(projp, False)
            ck([bstate, bstate_b])
            gla_pass(projo, True)
        except _Stop:
            finish()
            return nc

        if stop == 2:
            DS(lambda e: e.dma_start(out=S[0][:, :], in_=mixd[5]), r=[bmixd], w=[bS[0]], sem="S0")
            V(lambda e: e.tensor_copy(out=FS[0][:, :], in_=S[0][:, :]), r=[bS[0]], w=[bFS[0]])
            dump(FS[0][:, :], 1024, [bFS[0]])
            finish()
            return nc

        sqT = Hv[:, 0:16, :]
        TW = 1152
        skT = Q[:, 0:2 * TW].rearrange("p (a t) -> p a t", a=2)
        svT = Q[:, 2304:2304 + 2 * TW].rearrange("p (a t) -> p a t", a=2)
        so = 4608
        kdT = Q[:, so:so + 9 * 1024].rearrange("p (n g b t) -> p n g b t", n=9, g=4, b=2)
        so += 9 * 1024
        vaug = Q[:, so:so + 9 * 260].rearrange("p (n g d) -> p n g d", n=9, g=4)
        so += 9 * 260
        so = (so + 63) // 64 * 64
        qr_b = Q[:, so:so + 2048]
        so += 2048
        qTb = Q[:, so:so + 2048].rearrange("p (a t) -> p a t", a=16)
        so += 2048
        pexp = Q[:, so:so + 2048].rearrange("p (a t) -> p a t", a=16)
        so += 2048
        kdup = Q[:, so:so + 512].rearrange("p (g d) -> p g d", g=4)
        so += 512
        osn_b = Q[:, so:so + 2048]
        so += 2048
        mix2 = Q[:, so:so + 2048].rearrange("p (a t) -> p a t", a=16)
        so += 2048
        bkd, bva, bqr, bqT, bpex, bkdup, bosn, bmix2 = [Buf(n) for n in "kd va qr qT pex kdup osn mix2".split()]
        qt_f = X[:, 0:2048]
        tmp_f = X[:, 2048:4096]
        osw = X[:, 4096:6144]
        t2_f = X[:, 6144:8192]
        bqf, btf, bosw, bt2 = [Buf(n) for n in "qf tf osw t2".split()]
        ss32 = small[:, 32:64]
        den8 = small[:, 64:72]

        DSsplit(lambda a, b: sqT[:, a:b, :], lambda a, b: projo[49 + a:49 + b].rearrange("a p t -> p a t"), 16, 4, [bprojd], [bH], "H")
        DS(lambda e: e.dma_start(out=skT[:, :, 128:TW], in_=projo[65:67].rearrange("a p t -> p a t")), r=[bprojd], w=[bQ], sem="Q")
        DS(lambda e: e.dma_start(out=svT[:, :, 128:TW], in_=projo[67:69].rearrange("a p t -> p a t")), r=[bprojd], w=[bQ], sem="Q")
        DS(lambda e: e.dma_start(out=skT[:, :, 0:128], in_=projp[65:67, :, 0:128].rearrange("a p t -> p a t")), r=[bprojd], w=[bQ], sem="Q")
        DS(lambda e: e.dma_start(out=svT[:, :, 0:128], in_=projp[67:69, :, 0:128].rearrange("a p t -> p a t")), r=[bprojd], w=[bQ], sem="Q")
        V(lambda e: e.memset(vaug[:, :, :, 64:65], 1.0), w=[bva])
        V(lambda e: e.memset(Q[:, 4608:4608 + 9 * 1024], 0.0), r=[bQ], w=[bkd])

        def rope(xv, nh, blk, rb):
            cosb = cs_s[:, blk, 0, :].unsqueeze(1).to_broadcast([128, nh, 32])
            sinb = cs_s[:, blk, 1, :].unsqueeze(1).to_broadcast([128, nh, 32])
            x1 = xv[:, :, 0:32]
            x2 = xv[:, :, 32:64]
            ta = tmp_f[:, 0:nh * 32].rearrange("p (h d) -> p h d", h=nh)
            tb = tmp_f[:, 1024:1024 + nh * 32].rearrange("p (h d) -> p h d", h=nh)
            tc_ = t2_f[:, 0:nh * 32].rearrange("p (h d) -> p h d", h=nh)
            td = t2_f[:, 1024:1024 + nh * 32].rearrange("p (h d) -> p h d", h=nh)
            V(lambda e: e.tensor_tensor(out=ta, in0=x1, in1=cosb, op=ALU.mult), r=rb + [bconst], w=[btf])
            V(lambda e: e.tensor_tensor(out=tb, in0=x2, in1=sinb, op=ALU.mult), r=rb + [bconst], w=[btf])
            V(lambda e: e.tensor_tensor(out=tc_, in0=x2, in1=cosb, op=ALU.mult), r=rb + [bconst], w=[bt2])
            V(lambda e: e.tensor_tensor(out=td, in0=x1, in1=sinb, op=ALU.mult), r=rb + [bconst], w=[bt2])
            V(lambda e: e.tensor_tensor(out=x1, in0=ta, in1=tb, op=ALU.subtract), r=[btf], w=rb)
            V(lambda e: e.tensor_tensor(out=x2, in0=tc_, in1=td, op=ALU.add), r=[bt2], w=rb)

        def normrope(xf, nh, gain_b, blk, rb):
            xv = xf.rearrange("p (h d) -> p h d", h=nh)
            sq_ = tmp_f[:, 0:nh * 64]
            A(lambda e: e.activation(out=sq_, in_=xf, func=AF.Square), r=rb, w=[btf])
            ssx = ss32[:, 0:nh]
            V(lambda e: e.tensor_reduce(out=ssx, in_=sq_.rearrange("p (h d) -> p h d", h=nh), axis=AX.X, op=ALU.add), r=[btf], w=[bsmall])
            rstd_from(ssx, ssx, nh, 1.0 / 64, [bsmall], [bsmall])
            V(lambda e: e.tensor_tensor(out=xv, in0=xv, in1=ssx.unsqueeze(2).to_broadcast([128, nh, 64]), op=ALU.mult), r=rb + [bsmall], w=rb)
            if gain_b is not None:
                V(lambda e: e.tensor_tensor(out=xv, in0=xv, in1=gain_b.unsqueeze(1).to_broadcast([128, nh, 64]), op=ALU.mult), r=rb + [bconst], w=rb)
            if blk is not None:
                rope(xv, nh, blk, rb)

        ck2n = [0]

        def ck2(bufs):
            ck2n[0] += 1
            if stop == 200 + ck2n[0]:
                P.op("sync", None, reads=bufs)
                finish()
                raise StopBuild(nc)

        ck2([bH, bQ, bva, bkd])
        for n in range(9):
            ts_ = slice(n * 128, (n + 1) * 128)
            pbX = pb[0][:, :].bitcast(BF16)

            def trkv(e):
                for a in range(2):
                    e.transpose(out=pbX[:, a * 128:(a + 1) * 128], in_=skT[:, a, ts_], identity=ident_b[:, :])
                for a in range(2):
                    ins = e.transpose(out=pbX[:, 256 + a * 128:256 + (a + 1) * 128], in_=svT[:, a, ts_], identity=ident_b[:, :])
                return ins
            PE(trkv, r=[bQ, bconst], w=[bP[0]])
            ck2([bP[0]])
            kf = qt_f[:, 0:256]
            V(lambda e: e.tensor_copy(out=kf, in_=pbX[:, 0:256]), r=[bP[0]], w=[bqf])
            V(lambda e: e.tensor_copy(out=vaug[:, n, :, 0:64], in_=pbX[:, 256:512].rearrange("p (g d) -> p g d", g=4)), r=[bP[0]], w=[bva])
            ck2([bqf, bva])
            normrope(kf, 4, kn_b[:, :], n, [bqf])
            ck2([bqf, btf, bt2, bsmall])
            kfv = kf.rearrange("p (g d) -> p g d", g=4)
            V(lambda e: e.tensor_copy(out=kdup[:, :, 0:64], in_=kfv), r=[bqf], w=[bkdup])
            V(lambda e: e.tensor_copy(out=kdup[:, :, 64:128], in_=kfv), r=[bqf], w=[bkdup])
            pbY = pb[1][:, :].bitcast(BF16)

            def trkd(e):
                for g in range(4):
                    ins = e.transpose(out=pbY[:, g * 128:(g + 1) * 128], in_=kdup[:, g, :], identity=ident_b[:, :])
                return ins
            PE(trkd, r=[bkdup, bconst], w=[bP[1]])
            for hh in range(2):
                V(lambda e, hh=hh: e.tensor_copy(out=kdT[hh * 64:hh * 64 + 64, n, :, hh, :], in_=pbY[hh * 64:hh * 64 + 64, 0:512].rearrange("p (g t) -> p g t", g=4)), r=[bP[1]], w=[bkd])
            ck2([bkd, bkdup])
            if n == 0:
                continue
            j = n - 1
            to_ = slice(j * 128, (j + 1) * 128)
            pbQ2 = [pb[2][:, :].bitcast(BF16), pb[3][:, :].bitcast(BF16)]

            def trq(e):
                for a in range(16):
                    ins = e.transpose(out=pbQ2[a // 8][:, (a % 8) * 128:(a % 8 + 1) * 128], in_=sqT[:, a, to_], identity=ident_b[:, :])
                return ins
            PE(trq, r=[bH, bconst], w=[bP[2], bP[3]])
            V(lambda e: e.tensor_copy(out=qt_f[:, 0:1024], in_=pbQ2[0]), r=[bP[2]], w=[bqf])
            A(lambda e: e.activation(out=qt_f[:, 1024:2048], in_=pbQ2[1], func=AF.Copy), r=[bP[3]], w=[bqf])
            normrope(qt_f, 32, qn_b[:, :], n, [bqf])
            V(lambda e: e.tensor_copy(out=qr_b, in_=qt_f), r=[bqf], w=[bqr])

            def trqb(e):
                for a in range(16):
                    ins = e.transpose(out=pbQ2[a // 8][:, (a % 8) * 128:(a % 8 + 1) * 128], in_=qr_b[:, a * 128:(a + 1) * 128], identity=ident_b[:, :])
                return ins
            PE(trqb, r=[bqr, bconst], w=[bP[2], bP[3]])
            V(lambda e: e.tensor_copy(out=qTb[:, 0:8, :], in_=pbQ2[0].rearrange("p (a t) -> p a t", a=8)), r=[bP[2]], w=[bqT])
            A(lambda e: e.activation(out=qTb[:, 8:16, :], in_=pbQ2[1].rearrange("p (a t) -> p a t", a=8), func=AF.Copy), r=[bP[3]], w=[bqT])
            ck2([bqT, bqr, bqf])
            mprev = masks_f[:, 2, :] if j == 0 else masks_f[:, 1, :]
            for rd in range(4):
                ja = NADA0 + (n - 1) * 4 + rd
                if ja == NADA0:
                    ada_load(ja, 1, late=True)
                ada_comp(ja, 1, late=True)
                if ja + 1 < 48:
                    ada_load(ja + 1, 1, late=True)
                def smm2(e):
                    for hh in range(8):
                        h = 8 * rd + hh
                        p, r0 = h // 2, (h % 2) * 64
                        par, q_ = hh % 2, hh // 2
                        for kb in range(2):
                            slot = q_ * 2 + kb
                            ins = e.matmul(pb[4 + 2 * par + slot // 4][:, (slot % 4) * 128:(slot % 4 + 1) * 128], lhsT=kdT[:, n - 1 + kb, rd, h % 2, :], rhs=qTb[:, p, :], start=True, stop=True)
                    return ins
                PE(smm2, r=[bkd, bqT], w=[bP[4], bP[5], bP[6], bP[7]])
                ck2([bP[4], bP[5], bP[6], bP[7]])
                pe5 = pexp.rearrange("p (q r k) t -> p q r k t", r=2, k=2)
                for k in range(4):
                    A(lambda e, k=k: e.activation(out=pe5[:, 2 * (k % 2):2 * (k % 2) + 2, k // 2, :, :], in_=pb[4 + k][:, :].rearrange("p (q k t) -> p q k t", q=2, k=2), func=AF.Exp, scale=0.125), r=[bP[4 + k]], w=[bpex])
                pe4 = pexp.rearrange("p (h k) t -> p h k t", k=2)
                V(lambda e: e.tensor_tensor(out=pe4[:, :, 0, :], in0=pe4[:, :, 0, :], in1=mprev.unsqueeze(1).to_broadcast([128, 8, 128]), op=ALU.mult), r=[bpex, bconst], w=[bpex])
                V(lambda e: e.tensor_tensor(out=pe4[:, :, 1, :], in0=pe4[:, :, 1, :], in1=maskc.unsqueeze(1).to_broadcast([128, 8, 128]), op=ALU.mult), r=[bpex, bconst], w=[bpex])

                ck2([bpex])

                def pvmm(e):
                    for hh in range(8):
                        o_ap = pb[hh // 4][:, (hh % 4) * 65:(hh % 4) * 65 + 65]
                        for kb in range(2):
                            ins = e.matmul(o_ap, lhsT=pexp[:, hh * 2 + kb, :], rhs=vaug[:, n - 1 + kb, rd, :], start=(kb == 0), stop=(kb == 1))
                    return ins
                PE(pvmm, r=[bpex, bva], w=[bP[0], bP[1]])
                ck2([bP[0], bP[1]])
                for k in range(2):
                    ov = pb[k][:, 0:260].rearrange("p (h d) -> p h d", h=4)
                    dn = den8[:, 4 * k:4 * k + 4]
                    h0 = 8 * rd + 4 * k
                    V(lambda e, ov=ov, dn=dn, h0=h0: e.tensor_tensor(out=dn.unsqueeze(2), in0=ov[:, :, 64:65], in1=esink[:, h0:h0 + 4].unsqueeze(2), op=ALU.add), r=[bP[k], bconst], w=[bsmall])
                    V(lambda e, dn=dn: e.reciprocal(out=dn, in_=dn), r=[bsmall], w=[bsmall])
                    V(lambda e, ov=ov, dn=dn, h0=h0: e.tensor_tensor(out=osw[:, h0 * 64:(h0 + 4) * 64].rearrange("p (h d) -> p h d", h=4), in0=ov[:, :, 0:64], in1=dn.unsqueeze(2).to_broadcast([128, 4, 64]), op=ALU.mult), r=[bP[k], bsmall], w=[bosw])
            ck2([bosw, bsmall])
            normrope(osw, 32, None, None, [bosw])
            V(lambda e: e.tensor_copy(out=osn_b, in_=osw), r=[bosw], w=[bosn])

            def tro2(e):
                for a in range(16):
                    ins = e.transpose(out=pbQ2[a // 8][:, (a % 8) * 128:(a % 8 + 1) * 128], in_=osn_b[:, a * 128:(a + 1) * 128], identity=ident_b[:, :])
                return ins
            PE(tro2, r=[bosn, bconst], w=[bP[2], bP[3]])
            for k in range(2):
                V(lambda e, k=k: e.tensor_scalar(out=mix2[:, 8 * k:8 * k + 8, :], in0=pbQ2[k].rearrange("p (a t) -> p a t", a=8), scalar1=on_s[:, 0:1], scalar2=None, op0=ALU.mult), r=[bP[2 + k], bconst], w=[bmix2])
            DSsplit(lambda a, b: mixd[16 + a:16 + b, :, to_].rearrange("a p t -> p a t"), lambda a, b: mix2[:, a:b, :], 16, 4, [bmix2], [bmixd], "mix2")

        V(lambda e: e.tensor_scalar(out=modT[:, 128:160], in0=modT[:, 128:160], scalar1=1.0, scalar2=None, op0=ALU.add), r=[bmod], w=[bmod])
        ck2([bmixd])
        if stop == 3:
            for qi, src in enumerate([mixd[5], mixd[21], projo[3]]):
                DS(lambda e, src=src: e.dma_start(out=S[0][:, :], in_=src), r=[bmixd, bprojd], w=[bS[0]], sem="S0")
                V(lambda e: e.tensor_copy(out=FS[0], in_=S[0][:, :]), r=[bS[0]], w=[bFS[0]])
                DS(lambda e, qi=qi: e.dma_start(out=dbgo[:, qi * 1024:(qi + 1) * 1024], in_=FS[0]), r=[bFS[0]], w=[bdbg], sem="dbg")
            finish()
            return nc

        DSsplit(lambda a, b: Hv[:, a:b, :], lambda a, b: mixd[a:b].rearrange("a p t -> p a t"), 32, 4, [bmixd], [bH], "H")
        bx1d = Buf("x1d")
        sqb = Q[:, 0:1024]
        bsqb = Buf("sqb")

        def xo_load(i):
            s = i % 2
            DS(lambda e: e.dma_start(out=FS[s][:, :], in_=xT[:, i, 1024:2048]), w=[bFS[s]], sem=f"FS{s}")

        def op_consume(i, tag, banks):
            s = i % 2
            for q in range(2):
                V(lambda e, q=q: e.scalar_tensor_tensor(out=FS[s][:, q * 512:(q + 1) * 512], in0=pb[banks[q]][:, :], scalar=modT[:, G1 + i:G1 + i + 1], in1=FS[s][:, q * 512:(q + 1) * 512], op0=ALU.mult, op1=ALU.add), r=[bP[banks[q]], bmod, bFS[s]], w=[bFS[s]])
            A(lambda e: e.activation(out=sqb, in_=FS[s][:, :], func=AF.Square), r=[bFS[s]], w=[bsqb])

            def mm(e):
                for q in range(2):
                    ins = e.matmul(pb[6 + q][:, :], lhsT=ones_bf[:, :], rhs=sqb[:, q * 512:(q + 1) * 512], start=(i == 0), stop=(i == 31))
                return ins
            PE(mm, r=[bsqb, bconst], w=[bP[6], bP[7]])
            DS(lambda e: e.dma_start(out=x1d[i], in_=FS[s][:, :]), r=[bFS[s]], w=[bx1d], sem=f"FSo{s}")

        gemm([(w_out[j], j) for j in range(32)], Hv, [bH], FULL, op_consume, extra_load=xo_load)
        for q in range(2):
            rstd_from(pb[6 + q][:, :], rstd[:, q * 512:(q + 1) * 512], 512, 1.0 / D, [bP[6 + q]], [brstd])

        if stop == 4:
            DS(lambda e: e.dma_start(out=FS[0][:, :], in_=x1d[7]), r=[bx1d], w=[bFS[0]], sem="FS0")
            dump(FS[0][:, :], 1024, [bFS[0]])
            finish()
            return nc

        for i in range(32):
            s = i % 2
            DS(lambda e, i=i, s=s: e.dma_start(out=FS[s][:, :], in_=x1d[i]), r=[bx1d], w=[bFS[s]], sem=f"FS{s}")
            V(lambda e, s=s: e.tensor_tensor(out=FS[s][:, :], in0=FS[s][:, :], in1=rstd[:, :], op=ALU.mult), r=[bFS[s], brstd], w=[bFS[s]])
            V(lambda e, i=i, s=s: e.tensor_scalar(out=Hv[:, i, :], in0=FS[s][:, :], scalar1=modT[:, SC2 + i:SC2 + i + 1], scalar2=modT[:, SH2 + i:SH2 + i + 1], op0=ALU.mult, op1=ALU.add), r=[bFS[s], bmod], w=[bH])

        qTp = Q[:, 0:16384].rearrange("p (a t) -> p a t", a=16)
        bqTp = Buf("qTp")

        def q_consume(i, tag, banks):
            for q in range(2):
                if q == 0:
                    A(lambda e: e.activation(out=qTp[:, i, 0:512], in_=pb[banks[0]][:, :], func=AF.Copy), r=[bP[banks[0]]], w=[bqTp, bQ])
                else:
                    V(lambda e: e.tensor_copy(out=qTp[:, i, 512:1024], in_=pb[banks[1]][:, :]), r=[bP[banks[1]]], w=[bqTp, bQ])
        gemm([(w_q[j], j) for j in range(16)], Hv, [bH], FULL, q_consume)

        idxT = FSb[:, 0:3072].rearrange("p (f t) -> p f t", f=3)
        bidxT = Buf("idxT")
        s_sb = X[:, 0:2048].rearrange("p (f h k) -> p f h k", f=2, h=8)
        tmp128 = X[:, 2048:2176]
        cand = X[:, 2304:4352].rearrange("p (h a b) -> p h a b", h=8, a=16)
        tmp256 = X[:, 4352:4608]
        eq = X[:, 4608:6656].rearrange("p (h a b) -> p h a b", h=8, a=16)
        tk = X[:, 6656:7680]
        tki = X[:, 7680:8192].bitcast(U32)
        btk = Buf("tk")
        v12 = tk[:, 0:256].rearrange("p (f h k) -> p f h k", f=2, h=8)
        i12f = tk[:, 256:512].rearrange("p (f h k) -> p f h k", f=2, h=8)
        tv = tk[:, 512:640].rearrange("p (h k) -> p h k", h=8)
        kk = tk[:, 640:896].rearrange("p (f h k) -> p f h k", f=2, h=8)
        sel = tk[:, 896:1024]
        i12u = tki[:, 0:256].rearrange("p (f h k) -> p f h k", f=2, h=8)
        tpos = tki[:, 256:384].rearrange("p (h k) -> p h k", h=8)
        kku = tki[:, 384:512].rearrange("p (h k) -> p h k", h=8)
        res3 = sb("res3", [128, 3, 128], F32)
        bres = Buf("res3")
        bss, bcand, beq = Buf("s_sb"), Buf("cand"), Buf("eq")
        iota16 = iota_f[:, 0:16]
        NEG = -1e30

        def top16(src, nsrc, tmp, vout, iout, rb):
            V(lambda e: e.max(out=vout[:, 0:8], in_=src), r=rb, w=[btk])
            V(lambda e: e.max_index(out=iout[:, 0:8], in_max=vout[:, 0:8], in_values=src), r=rb + [btk], w=[btk])
            V(lambda e: e.match_replace(out=tmp, in_to_replace=vout[:, 0:8], in_values=src, imm_value=NEG), r=rb + [btk], w=[bX])
            V(lambda e: e.max(out=vout[:, 8:16], in_=tmp), r=[bX], w=[btk])
            V(lambda e: e.max_index(out=iout[:, 8:16], in_max=vout[:, 8:16], in_values=tmp), r=[bX, btk], w=[btk])

        for tt in range(8):
            tsl = slice(tt * 128, (tt + 1) * 128)

            def smm3(e):
                for f in range(2):
                    for h in range(8):
                        ins = e.matmul(pb[f * 2 + h // 4][:, (h % 4) * 128:(h % 4 + 1) * 128], lhsT=qTp[:, h * 2 + f, tsl], rhs=keys_b[:, f, :], start=True, stop=True)
                return ins
            PE(smm3, r=[bqTp, bconst], w=[bP[0], bP[1], bP[2], bP[3]])
            for k in range(4):
                dst = X[:, k * 512:(k + 1) * 512]
                if k % 2 == 0:
                    V(lambda e, k=k, dst=dst: e.tensor_copy(out=dst, in_=pb[k][:, :]), r=[bP[k]], w=[bX])
                else:
                    A(lambda e, k=k, dst=dst: e.activation(out=dst, in_=pb[k][:, :], func=AF.Copy), r=[bP[k]], w=[bX])
            for f in range(2):
                for h in range(8):
                    top16(s_sb[:, f, h, :], 128, tmp128, v12[:, f, h, :], i12u[:, f, h, :], [bX])
            V(lambda e: e.tensor_copy(out=tk[:, 256:512], in_=tki[:, 0:256]), r=[btk], w=[btk])
            V(lambda e: e.tensor_tensor(out=cand, in0=v12[:, 0, :, :].unsqueeze(3).to_broadcast([128, 8, 16, 16]), in1=v12[:, 1, :, :].unsqueeze(2).to_broadcast([128, 8, 16, 16]), op=ALU.add), r=[btk], w=[bX])
            for h in range(8):
                top16(cand[:, h, :, :].rearrange("p a b -> p (a b)"), 256, tmp256, tv[:, h, :], tpos[:, h, :], [bX])
            gate = res3[:, 2, :].rearrange("p (h k) -> p h k", h=8)
            V(lambda e: e.tensor_tensor(out=gate, in0=tv, in1=tv[:, :, 0:1].to_broadcast([128, 8, 16]), op=ALU.subtract), r=[btk], w=[bres])
            A(lambda e: e.activation(out=gate, in_=gate, func=AF.Exp), r=[bres], w=[bres])
            gs8 = small[:, 80:88]
            V(lambda e: e.tensor_reduce(out=gs8, in_=gate, axis=AX.X, op=ALU.add), r=[bres], w=[bsmall])
            V(lambda e: e.reciprocal(out=gs8, in_=gs8), r=[bsmall], w=[bsmall])
            V(lambda e: e.tensor_tensor(out=gate, in0=gate, in1=gs8.unsqueeze(2).to_broadcast([128, 8, 16]), op=ALU.mult), r=[bres, bsmall], w=[bres])
            V(lambda e: e.tensor_single_scalar(out=kku, in_=tpos, scalar=4, op=ALU.logical_shift_right), r=[btk], w=[btk])
            V(lambda e: e.tensor_copy(out=kk[:, 0, :, :], in_=kku), r=[btk], w=[btk])
            V(lambda e: e.tensor_single_scalar(out=kku, in_=tpos, scalar=15, op=ALU.bitwise_and), r=[btk], w=[btk])
            V(lambda e: e.tensor_copy(out=kk[:, 1, :, :], in_=kku), r=[btk], w=[btk])
            for f in range(2):
                V(lambda e, f=f: e.tensor_tensor(out=eq, in0=kk[:, f, :, :].unsqueeze(3).to_broadcast([128, 8, 16, 16]), in1=iota16.unsqueeze(1).unsqueeze(1).to_broadcast([128, 8, 16, 16]), op=ALU.is_equal), r=[btk, bconst], w=[bX])
                V(lambda e, f=f: e.tensor_tensor(out=eq, in0=eq, in1=i12f[:, f, :, :].unsqueeze(2).to_broadcast([128, 8, 16, 16]), op=ALU.mult), r=[bX, btk], w=[bX])
                V(lambda e, f=f: e.tensor_reduce(out=res3[:, f, :].rearrange("p (h k) -> p h k", h=8), in_=eq, axis=AX.X, op=ALU.add), r=[bX], w=[bres])

            def tr3(e):
                for f in range(3):
                    ins = e.transpose(out=pb[4][:, f * 128:(f + 1) * 128], in_=res3[:, f, :], identity=ident_f[:, :])
                return ins
            PE(tr3, r=[bres, bconst], w=[bP[4]])
            V(lambda e: e.tensor_copy(out=idxT[:, :, tsl], in_=pb[4][:, 0:384].rearrange("p (f t) -> p f t", f=3)), r=[bP[4]], w=[bidxT, bFS[0], bFS[1]])

        if stop == 5:
            V(lambda e: e.tensor_copy(out=X[:, 0:3072], in_=FSb[:, 0:3072]), r=[bidxT], w=[bX])
            DS(lambda e: e.dma_start(out=dbgo[:, 0:3072], in_=X[:, 0:3072]), r=[bX], w=[bdbg], sem="dbg")
            for qi, src in enumerate([mixd[5], mixd[21]]):
                DS(lambda e, src=src: e.dma_start(out=S[0][:, :], in_=src), r=[bmixd, bprojd], w=[bS[0]], sem="S0")
                V(lambda e, qi=qi: e.tensor_copy(out=X[:, 4096 + qi * 1024:4096 + (qi + 1) * 1024], in_=S[0][:, :]), r=[bS[0]], w=[bX])
            DS(lambda e: e.dma_start(out=dbgo[:, 3072:5120], in_=X[:, 4096:6144]), r=[bX], w=[bdbg], sem="dbg")
            DS(lambda e: e.dma_start(out=X[:, 6144:7168], in_=x1d[7]), r=[bx1d], w=[bX], sem="X")
            DS(lambda e: e.dma_start(out=dbgo[:, 5120:6144], in_=X[:, 6144:7168]), r=[bX], w=[bdbg], sem="dbg")
            finish()
            return nc

        Gblk = Q[:, 0:16384].rearrange("p (i t) -> p i t", t=128)
        Aoh = [Q[:, 16384 + b_ * 4096:16384 + b_ * 4096 + 2048].rearrange("p (t i) -> p t i", i=128) for b_ in range(2)]
        Boh = [Q[:, 16384 + b_ * 4096 + 2048:16384 + b_ * 4096 + 4096].rearrange("p (t i) -> p t i", i=128) for b_ in range(2)]
        bG = Buf("Gblk")
        bA = [Buf("Aoh0"), Buf("Aoh1")]
        bB = [Buf("Boh0"), Buf("Boh1")]
        bGd = Buf("Gd")
        bGdq = [Buf(f"Gd{q}") for q in range(32)]
        iob = iota_f[:, :].unsqueeze(1).to_broadcast([128, 16, 128])
        for tb in range(8):
            for sub in range(8):
                b_ = sub % 2
                t0 = tb * 128 + sub * 16
                V(lambda e: e.tensor_tensor(out=Aoh[b_], in0=iob, in1=idxT[:, 0, t0:t0 + 16].unsqueeze(2).to_broadcast([128, 16, 128]), op=ALU.is_equal), r=[bidxT, bconst], w=[bA[b_]])
                V(lambda e: e.tensor_tensor(out=Boh[b_], in0=iob, in1=idxT[:, 1, t0:t0 + 16].unsqueeze(2).to_broadcast([128, 16, 128]), op=ALU.is_equal), r=[bidxT, bconst], w=[bB[b_]])
                V(lambda e: e.tensor_tensor(out=Boh[b_], in0=Boh[b_], in1=idxT[:, 2, t0:t0 + 16].unsqueeze(2).to_broadcast([128, 16, 128]), op=ALU.mult), r=[bidxT, bB[b_]], w=[bB[b_]])
                bks = [4 * b_ + k for k in range(4)]

                def gmm(e):
                    for t in range(16):
                        ins = e.matmul(pb[bks[t // 4]][:, (t % 4) * 128:(t % 4 + 1) * 128], lhsT=Aoh[b_][:, t, :], rhs=Boh[b_][:, t, :], start=True, stop=True)
                    return ins
                PE(gmm, r=[bA[b_], bB[b_]], w=[bP[k_] for k_ in bks])
                for k in range(4):
                    tl0 = sub * 16 + k * 4
                    dst = Gblk[:, :, tl0:tl0 + 4].rearrange("p i t -> p t i")
                    src = pb[bks[k]][:, :].rearrange("p (t i) -> p t i", t=4)
                    A(lambda e, dst=dst, src=src: e.activation(out=dst, in_=src, func=AF.Copy), r=[bP[bks[k]]], w=[bG])
            for q in range(32):
                DS(lambda e, q=q, tb=tb: e.dma_start(out=Gd[4 * q:4 * q + 4, :, tb * 128:(tb + 1) * 128].rearrange("j p t -> p j t"), in_=Gblk[:, 4 * q:4 * q + 4, :]), r=[bG], w=[bGdq[q]], sem=f"Gd{q}")

        if stop == 6:
            for qi, jj_ in enumerate([5, 77]):
                DS(lambda e, jj_=jj_: e.dma_start(out=S[0][:, :], in_=Gd[jj_]), r=bGdq, w=[bS[0]], sem="S0")
                V(lambda e, qi=qi: e.tensor_copy(out=X[:, qi * 1024:(qi + 1) * 1024], in_=S[0][:, :]), r=[bS[0]], w=[bX])
            V(lambda e: e.tensor_copy(out=X[:, 2048:5120], in_=FSb[:, 0:3072]), r=[bidxT], w=[bX])
            DS(lambda e: e.dma_start(out=dbgo[:, 0:5120], in_=X[:, 0:5120]), r=[bX], w=[bdbg], sem="dbg")
            finish()
            return nc

        bhTd = Buf("hTd")
        GS = [Q[:, 24576 + i * 1024:24576 + (i + 1) * 1024] for i in range(3)]
        bGS = [Buf(f"GS{i}") for i in range(3)]

        def g_load(i):
            s = i % 3
            DS(lambda e: e.dma_start(out=GS[s], in_=Gd[i]), r=[bGdq[i // 4]], w=[bGS[s]], sem=f"GS{s}")

        def p1_consume(i, tag, banks):
            s = i % 3
            for q in range(2):
                A(lambda e, q=q: e.activation(out=S[s][:, q * 512:(q + 1) * 512], in_=pb[banks[q]][:, :], func=AF.Gelu), r=[bP[banks[q]]], w=[bS[s]])
            V(lambda e: e.tensor_tensor(out=S[s][:, :], in0=S[s][:, :], in1=GS[s], op=ALU.mult), r=[bS[s], bGS[s]], w=[bS[s]])
            DS(lambda e: e.dma_start(out=hTd[i], in_=S[s][:, :]), r=[bS[s]], w=[bhTd], sem=f"S{s}")

        gemm([(down[j], j) for j in range(128)], Hv, [bH], FULL, p1_consume, extra_load=g_load)

        if stop == 7:
            for qi, jj_ in enumerate([5, 77]):
                DS(lambda e, jj_=jj_: e.dma_start(out=S[0][:, :], in_=hTd[jj_]), r=[bhTd], w=[bS[0]], sem="S0")
                V(lambda e, qi=qi: e.tensor_copy(out=X[:, qi * 1024:(qi + 1) * 1024], in_=S[0][:, :]), r=[bS[0]], w=[bX])
            DS(lambda e: e.dma_start(out=dbgo[:, 0:2048], in_=X[:, 0:2048]), r=[bX], w=[bdbg], sem="dbg")
            finish()
            return nc

        HS = [Q[:, i * 8192:(i + 1) * 8192].rearrange("p (j t) -> p j t", j=8) for i in range(3)]
        bHS = [Buf(f"HS{i}") for i in range(3)]
        kq = [0]
        NRES = 6
        Xb = X[:, :].bitcast(BF16)
        RES = [H[:, i * 8192:(i + 1) * 8192].rearrange("p (j t) -> p j t", j=8) for i in range(4)] + [Xb[:, i * 8192:(i + 1) * 8192].rearrange("p (j t) -> p j t", j=8) for i in range(2)]
        bRES = [Buf(f"RES{i}") for i in range(NRES)]
        for s_ in range(8):
            def p2_load(jg):
                k = kq[0] + jg
                sl = k % 3
                sw = k % 2
                DG(lambda e: e.dma_start(out=W[sw][:, :], in_=up[s_, jg], max_dma_last_dim=8192), w=[bW[sw]], sem=f"W{sw}")
                if jg < NRES:
                    if s_ == 0:
                        for hf in range(2):
                            DS(lambda e, hf=hf: e.dma_start(out=RES[jg][:, 4 * hf:4 * hf + 4, :], in_=hTd[jg * 8 + 4 * hf:jg * 8 + 4 * hf + 4].rearrange("j p t -> p j t")), r=[bhTd], w=[bRES[jg]] + ([bH] if jg < 4 else [bX]), sem=f"RES{jg}")
                    return
                for hf in range(2):
                    DS(lambda e, hf=hf: e.dma_start(out=HS[sl][:, 4 * hf:4 * hf + 4, :], in_=hTd[jg * 8 + 4 * hf:jg * 8 + 4 * hf + 4].rearrange("j p t -> p j t")), r=[bhTd], w=[bHS[sl], bQ], sem=f"HS{sl}")

            def p2_comp(jg):
                k = kq[0] + jg
                sl = k % 3
                sw = k % 2
                Wv = W[sw][:, :].rearrange("p (j d) -> p j d", j=8)
                hsrc = RES[jg] if jg < NRES else HS[sl]
                hbuf = bRES[jg] if jg < NRES else bHS[sl]

                def mm(e):
                    for jj in range(8):
                        for dt in range(4):
                            for th in range(2):
                                ins = e.matmul(pb[dt * 2 + th][:, :], lhsT=Wv[:, jj, dt * 128:(dt + 1) * 128], rhs=hsrc[:, jj, th * 512:(th + 1) * 512], start=(jg == 0 and jj == 0), stop=(jg == 15 and jj == 7))
                    return ins
                PE(mm, r=[bW[sw], hbuf], w=bP)
            for i in range(16 + 1):
                if i < 16:
                    p2_load(i)
                if i - 1 >= 0:
                    p2_comp(i - 1)
            kq[0] += 16
            if stop == 8:
                for k in range(4):
                    V(lambda e, k=k: e.tensor_copy(out=X[:, k * 512:(k + 1) * 512], in_=pb[k][:, :]), r=[bP[k]], w=[bX])
                V(lambda e: e.tensor_copy(out=X[:, 2048:3072], in_=HS[0][:, 0, :]), r=[bHS[0]], w=[bX])
                V(lambda e: e.tensor_copy(out=X[:, 3072:4096], in_=W[0][:, 0:1024]), r=[bW[0]], w=[bX])
                DS(lambda e: e.dma_start(out=dbgo[:, 0:4096], in_=X[:, 0:4096]), r=[bX], w=[bdbg], sem="dbg")
                finish()
                return nc
            for dt in range(4):
                di = s_ * 4 + dt
                s = di % 2
                DS(lambda e, di=di, s=s: e.dma_start(out=FS[s][:, :], in_=x1d[di]), r=[bx1d], w=[bFS[s]], sem=f"FS{s}")
                for th in range(2):
                    V(lambda e, di=di, s=s, th=th, dt=dt: e.scalar_tensor_tensor(out=FS[s][:, th * 512:(th + 1) * 512], in0=pb[dt * 2 + th][:, :], scalar=modT[:, G2 + di:G2 + di + 1], in1=FS[s][:, th * 512:(th + 1) * 512], op0=ALU.mult, op1=ALU.add), r=[bP[dt * 2 + th], bmod, bFS[s]], w=[bFS[s]])
                DS(lambda e, di=di, s=s: e.dma_start(out=outT[:, di, :], in_=FS[s][:, :]), r=[bFS[s]], w=[bout], sem=f"FSo{s}")
        finish()
    return nc


def _tile_w(w, ncol):
    N = w.shape[1]
    nt = N // ncol
    return np.ascontiguousarray(w.reshape(NCH, 128, nt, ncol).transpose(2, 1, 0, 3)).reshape(nt, 128, NCH * ncol)


def prep_shared(inp):
    f = np.float32
    sh = {}
    sh["w_ada"] = _tile_w(np.asarray(inp["w_ada"][0], f), 512)
    sh["b_ada"] = np.ascontiguousarray(np.asarray(inp["b_ada"], f).reshape(1, 24576))
    w_in = np.asarray(inp["w_in"][0], f)
    ga = np.zeros((D, 128), f)
    ga[:, :16] = w_in[:, 6144:6160]
    w_in_p = np.concatenate([w_in[:, :6144], ga, w_in[:, 6160:]], axis=1)
    sh["w_in"] = _tile_w(w_in_p, 128)
    sh["wg"] = np.ascontiguousarray(np.asarray(inp["w_gla_gate_up"][0], f))
    sh["bg"] = np.ascontiguousarray(np.asarray(inp["b_gla_gate"], f).reshape(1, 1024))
    sh["gla_gain"] = np.ascontiguousarray(np.asarray(inp["gla_out_norm"][0], f).reshape(128, 1))
    sh["qn"] = np.ascontiguousarray(np.asarray(inp["swa_q_norm"][0], f))
    sh["kn"] = np.ascontiguousarray(np.asarray(inp["swa_k_norm"][0], f))
    on = np.asarray(inp["swa_out_norm"][0], f)
    sh["on"] = np.ascontiguousarray(np.concatenate([on, on]).reshape(128, 1))
    sh["sinks"] = np.ascontiguousarray(np.asarray(inp["swa_sinks"][0], f))
    sh["w_out"] = _tile_w(np.asarray(inp["w_out"][0], f), 128)
    sh["w_q"] = _tile_w(np.asarray(inp["w_peer_q"][0], f), 128)
    k1 = np.asarray(inp["peer_sub_keys_1"][0], f)
    k2 = np.asarray(inp["peer_sub_keys_2"][0], f)
    sh["keysT"] = np.ascontiguousarray(np.stack([k1.T, k2.T], axis=1))
    dn = np.asarray(inp["peer_expert_down"][0], f)
    sh["down"] = np.ascontiguousarray(dn.reshape(128, 128, NCH, 128).transpose(1, 3, 2, 0)).reshape(128, 128, 4096)
    upw = np.asarray(inp["peer_expert_up"][0], f)
    u = upw.reshape(128, 16, 8, 8, 512).transpose(3, 1, 0, 2, 4)
    sh["up"] = np.ascontiguousarray(u).reshape(8, 16, 128, 4096)
    s_ = np.arange(128)[:, None]
    t_ = np.arange(128)[None, :]
    sh["_mcur"] = (s_ <= t_).astype(f)
    sh["_mprev"] = (s_ > t_).astype(f)
    sh["ident"] = np.eye(128, dtype=f)
    sh["iotar"] = np.ascontiguousarray(np.broadcast_to(np.arange(128, dtype=f)[None, :], (128, 128)))
    return sh


def prep_core(inp, sh, r):
    f = np.float32
    b, half = r // 2, r % 2
    x = np.asarray(inp["x"], f)
    if half == 1:
        xw = x[b]
    else:
        xw = np.concatenate([np.zeros((1024, D), f), x[b, :1024]], axis=0)
    m = {k: v for k, v in sh.items() if not k.startswith("_")}
    m["xT"] = np.ascontiguousarray(xw.reshape(2048, NCH, 128).transpose(2, 1, 0))
    m["cT"] = np.ascontiguousarray(np.asarray(inp["c"], f)[b].reshape(NCH, 128).T)
    pos = (half * 1024 - 128 + np.arange(1152)).astype(f)
    inv = (10000.0 ** (-np.arange(32, dtype=np.float64) / 32)).astype(f)
    ang = pos[:, None] * inv[None, :]
    cs = np.stack([np.cos(ang), np.sin(ang)], axis=1).astype(f)
    m["cs"] = np.ascontiguousarray(cs.reshape(9, 128, 2, 32).transpose(1, 0, 2, 3))
    fl = float(half)
    m["masks"] = np.ascontiguousarray(np.stack([sh["_mcur"], sh["_mprev"], sh["_mprev"] * fl], axis=1))
    m["flag"] = np.full((128, 1), fl, f)
    return m


_NC_CACHE = {}


def kernel(**inputs):
    sh = prep_shared(inputs)
    in_maps = [prep_core(inputs, sh, r) for r in range(N_CORES)]
    if "nc" not in _NC_CACHE:
        _NC_CACHE["nc"] = build_nc()
    nc = _NC_CACHE["nc"]
    res = run_bass_kernel_spmd(nc, in_maps, core_ids=list(range(N_CORES)))
    out = np.empty((4, 2048, D), np.float32)
    for r in range(N_CORES):
        b, half = r // 2, r % 2
        o = np.asarray(res.results[r]["outT"])
        out[b, half * 1024:(half + 1) * 1024, :] = o.transpose(2, 1, 0).reshape(1024, D)
    return out
```

```python
import numpy as np
from contextlib import ExitStack
import concourse.bass as bass
import concourse.mybir as mybir
from concourse.bass_utils import run_bass_kernel_spmd

F32 = mybir.dt.float32
BF16 = mybir.dt.bfloat16
U32 = mybir.dt.uint32
AF = mybir.ActivationFunctionType
ALU = mybir.AluOpType
AX = mybir.AxisListType

D = 4096
NCH = 32
T = 1024
EPS = 1e-6
N_CORES = 8


class StopBuild(Exception):
    def __init__(self, nc):
        self.nc = nc


class Buf:
    __slots__ = ("name", "w", "r", "excl")

    def __init__(self, name, excl=False):
        self.name = name
        self.w = None
        self.r = []
        self.excl = excl


class _Rec:
    def __init__(self):
        self.calls = []

    def __getattr__(self, name):
        def f(*a, **k):
            self.calls.append((name, a, k))
            return self
        return f


class Prog:
    ENG = ("tensor", "vector", "scalar", "gpsimd", "sync")

    def __init__(self, nc, es):
        self.nc = nc
        self.es = es
        self.ops = {e: [] for e in self.ENG}
        self.cnt = {}
        self.waited = {e: {} for e in self.ENG}
        self.sems = {}
        for e in self.ENG:
            self.sem(("eng", e))

    def sem(self, key):
        if key not in self.sems:
            name = "s_" + "_".join(str(k) for k in key) if isinstance(key, tuple) else "s_" + str(key)
            self.sems[key] = self.es.enter_context(self.nc.semaphore(name))
            self.cnt[key] = 0
        return key

    def op(self, eng, fn, reads=(), writes=(), dma=None):
        deps = {}

        def add(ev, is_raw):
            if ev is None:
                return
            k, v = ev
            if dma is None and k == ("eng", eng):
                if eng == "tensor" or not is_raw:
                    return
            if deps.get(k, 0) < v:
                deps[k] = v

        for b in reads:
            add(b.w, True)
            if b.excl:
                for ev in b.r:
                    add(ev, False)
        for b in writes:
            add(b.w, False)
            for ev in b.r:
                add(ev, False)
        wt = self.waited[eng]
        waits = []
        for k, v in deps.items():
            if wt.get(k, 0) < v:
                wt[k] = v
                waits.append((k, v))
        if dma is None:
            key = ("eng", eng)
            self.cnt[key] += 1
            inc = 1
        else:
            key = self.sem(dma)
            self.cnt[key] += 16
            inc = 16
        ev = (key, self.cnt[key])
        for b in reads:
            b.r.append(ev)
            if len(b.r) > 64:
                m = {}
                for k, v in b.r:
                    if m.get(k, 0) < v:
                        m[k] = v
                b.r = list(m.items())
        for b in writes:
            b.w = ev
            b.r = []
        calls = None
        if fn is not None:
            rec = _Rec()
            fn(rec)
            calls = rec.calls
        self.ops[eng].append((waits, calls, key, inc))
        return ev

    def emit(self):
        nc = self.nc
        with nc.Block() as block:
            for e in self.ENG:
                ops = self.ops[e]
                if not ops:
                    continue

                def body(engine, ops=ops):
                    for waits, fn, key, inc in ops:
                        for k, v in waits:
                            engine.wait_ge(self.sems[k], v)
                        if fn is None:
                            ins = engine.nop()
                        else:
                            for name, a, kw in fn:
                                ins = getattr(engine, name)(*a, **kw)
                        ins.then_inc(self.sems[key], inc)

                getattr(block, e)(body)


def build_nc(stop=10**9, dbg=False):
    nc = bass.Bass("TRN2", target_bir_lowering=False)
    es = ExitStack()

    def din(name, shape, dt=F32):
        return nc.dram_tensor(name, list(shape), dt, kind="ExternalInput").ap()

    def dscr(name, shape, dt):
        return nc.dram_tensor(name, list(shape), dt).ap()

    xT = din("xT", [128, NCH, 2048])
    cT = din("cT", [128, NCH])
    w_ada = din("w_ada", [48, 128, NCH * 512])
    b_ada = din("b_ada", [1, 24576])
    w_in = din("w_in", [69, 128, 4096])
    wg = din("wg", [16, 1024])
    bg = din("bg", [1, 1024])
    gla_gain = din("gla_gain", [128, 1])
    qn = din("qn", [64])
    kn = din("kn", [64])
    on = din("on", [128, 1])
    sinks = din("sinks", [32])
    cs = din("cs", [128, 9, 2, 32])
    w_out = din("w_out", [32, 128, 4096])
    w_q = din("w_q", [16, 128, 4096])
    keysT = din("keysT", [128, 2, 128])
    down = din("down", [128, 128, 4096])
    up = din("up", [8, 16, 128, 4096])
    masks = din("masks", [128, 3, 128])
    ident = din("ident", [128, 128])
    iotar = din("iotar", [128, 128])
    flag = din("flag", [128, 1])
    outT = nc.dram_tensor("outT", [128, NCH, T], F32, kind="ExternalOutput").ap()
    dbgo = None
    if dbg:
        dbgo = nc.dram_tensor("dbgo", [128, 8192], F32, kind="ExternalOutput").ap()

    projo = dscr("projo", [69, 128, T], BF16)
    projp = dscr("projp", [69, 128, T], BF16)
    mixd = dscr("mixd", [32, 128, T], BF16)
    x1d = dscr("x1d", [32, 128, T], F32)
    Gd = dscr("Gd", [128, 128, T], BF16)
    hTd = dscr("hTd", [128, 128, T], BF16)

    with es:
        P = Prog(nc, es)

        def sb(name, shape, dt):
            return es.enter_context(nc.sbuf_tensor(name, list(shape), dt))

        H = sb("H", [128, 32768], BF16)
        Q = sb("Q", [128, 28672], BF16)
        X = sb("X", [128, 8192], F32)
        W = [sb(f"W{i}", [128, 4096], BF16) for i in range(2)]
        S = [sb(f"S{i}", [128, 1024], BF16) for i in range(3)]
        FSA = sb("FSA", [128, 2048], F32)
        FS = [FSA[:, 0:1024], FSA[:, 1024:2048]]
        bH, bQ, bX = Buf("H"), Buf("Q"), Buf("X")
        bW = [Buf(f"W{i}") for i in range(2)]
        bS = [Buf(f"S{i}") for i in range(3)]
        bFS = [Buf(f"FS{i}") for i in range(2)]
        pb = [es.enter_context(nc.psum_tensor(f"pb{i}", [128, 512], F32)) for i in range(8)]
        bP = [Buf(f"pb{i}", excl=True) for i in range(8)]

        modT = sb("modT", [128, 192], F32)
        bmod = Buf("modT")
        c_f = sb("c_f", [128, NCH], F32)
        c_bf = sb("c_bf", [128, NCH], BF16)
        bc = Buf("c")
        bbrow = [Buf("brow")] * 2
        bmrow = [Buf("mrow")] * 2
        one11 = sb("one11", [1, 1], F32)
        ones_bf = sb("ones_bf", [128, 128], BF16)
        ident_f = sb("ident_f", [128, 128], F32)
        ident_b = sb("ident_b", [128, 128], BF16)
        iota_f = sb("iota_f", [128, 128], F32)
        masks_f = sb("masks_f", [128, 3, 128], F32)
        ltri = sb("ltri", [128, 128], F32)
        flag_s = sb("flag_s", [128, 1], F32)
        gain_s = sb("gain_s", [128, 1], F32)
        on_s = sb("on_s", [128, 1], F32)
        FSb = FSA[:, :].bitcast(BF16)
        wg_b = FSb[0:16, 0:1024]
        bg_b = FSb[0:1, 1024:2048]
        bwg = Buf("wg")
        keys_b = sb("keys_b", [128, 2, 128], BF16)
        qn_b = sb("qn_b", [128, 64], F32)
        kn_b = sb("kn_b", [128, 64], F32)
        esink = sb("esink", [128, 32], F32)
        cs_s = sb("cs_s", [128, 9, 2, 32], F32)
        bconst = Buf("const")
        rstd = sb("rstd", [128, 1024], F32)
        brstd = Buf("rstd")
        bm_t = rstd[0:1, :]
        brow = [bm_t[0:1, 0:512], bm_t[0:1, 0:512]]
        mrow = [bm_t[0:1, 512:1024], bm_t[0:1, 512:1024]]
        state = sb("state", [128, 8, 128], F32)
        state_b = sb("state_b", [128, 8, 2, 128], BF16)
        bstate, bstate_b = Buf("state"), Buf("state_b")
        small = sb("small", [128, 256], F32)
        bsmall = Buf("small")

        def V(fn, r=(), w=()):
            return P.op("vector", fn, r, w)

        def A(fn, r=(), w=()):
            return P.op("scalar", fn, r, w)

        def PE(fn, r=(), w=()):
            return P.op("tensor", fn, r, w)

        def DS(fn, r=(), w=(), sem=None):
            return P.op("sync", fn, r, w, dma=sem)

        def DG(fn, r=(), w=(), sem=None):
            return P.op("gpsimd", fn, r, w, dma=sem)

        def DSsplit(dst_tiles, src_tiles, n, step, r, w, sem):
            for a in range(0, n, step):
                b_ = min(n, a + step)
                DS(lambda e, a=a, b_=b_: e.dma_start(out=dst_tiles(a, b_), in_=src_tiles(a, b_)), r=r, w=w, sem=sem)

        DS(lambda e: e.dma_start(out=ident_f[:, :], in_=ident), w=[bconst], sem="c0")
        DS(lambda e: e.dma_start(out=iota_f[:, :], in_=iotar), w=[bconst], sem="c0")
        DS(lambda e: e.dma_start(out=masks_f[:, :, :], in_=masks), w=[bconst], sem="c0")
        DS(lambda e: e.dma_start(out=flag_s[:, :], in_=flag), w=[bconst], sem="c0")
        DS(lambda e: e.dma_start(out=gain_s[:, :], in_=gla_gain), w=[bconst], sem="c0")
        DS(lambda e: e.dma_start(out=on_s[:, :], in_=on), w=[bconst], sem="c0")
        DS(lambda e: e.dma_start(out=cs_s[:, :, :, :], in_=cs), w=[bconst], sem="c0")
        DS(lambda e: e.dma_start(out=qn_b[:, :], in_=qn.partition_broadcast(128)), w=[bconst], sem="c0")
        DS(lambda e: e.dma_start(out=kn_b[:, :], in_=kn.partition_broadcast(128)), w=[bconst], sem="c0")
        DS(lambda e: e.dma_start(out=esink[:, :], in_=sinks.partition_broadcast(128)), w=[bconst], sem="c0")
        DS(lambda e: e.dma_start(out=c_f[:, :], in_=cT), w=[bc], sem="c2")
        DG(lambda e: e.dma_start(out=keys_b[:, :, :], in_=keysT), w=[bconst], sem="c1")
        V(lambda e: e.tensor_copy(out=ident_b[:, :], in_=ident_f[:, :]), r=[bconst], w=[bconst])
        V(lambda e: e.memset(ones_bf[:, :], 1.0), w=[bconst])
        V(lambda e: e.memset(one11[:, :], 1.0), w=[bconst])
        V(lambda e: e.tensor_scalar(out=ltri[:, :], in0=masks_f[:, 0, :], scalar1=-1.0 / 16.0, scalar2=None, op0=ALU.mult), r=[bconst], w=[bconst])
        A(lambda e: e.activation(out=esink[:, :], in_=esink[:, :], func=AF.Exp), r=[bconst], w=[bconst])
        A(lambda e: e.activation(out=c_f[:, :], in_=c_f[:, :], func=AF.Silu), r=[bc], w=[bc])
        V(lambda e: e.tensor_copy(out=c_bf[:, :], in_=c_f[:, :]), r=[bc], w=[bc])

        Hh = [H[:, 0:16384], H[:, 16384:32768]]
        bHh = [Buf("Hh0"), Buf("Hh1")]
        NADA = 48

        def ada_load(j, s, late=False):
            if late:
                DG(lambda e: e.dma_start(out=Hh[s], in_=w_ada[j], max_dma_last_dim=8192), r=[bH], w=[bHh[s]], sem=f"Hh{s}")
            else:
                DG(lambda e: e.dma_start(out=Hh[s], in_=w_ada[j], max_dma_last_dim=8192), w=[bHh[s], bH], sem=f"Hh{s}")

        def ada_comp(j, s, late=False):
            pa = pb[6] if late else pb[s]
            bpa = bP[6] if late else bP[s]
            DS(lambda e: e.dma_start(out=brow[0], in_=b_ada[:, j * 512:(j + 1) * 512]), w=[bbrow[0], brstd], sem="brow")

            def mm(e):
                for c in range(NCH):
                    ins = e.matmul(pa[0:1, :], lhsT=c_bf[:, c:c + 1], rhs=Hh[s][:, c * 512:(c + 1) * 512], start=(c == 0), stop=(c == NCH - 1))
                return ins
            PE(mm, r=[bHh[s], bc] + ([bH] if late else []), w=[bpa])
            V(lambda e: e.tensor_tensor(out=mrow[0], in0=pa[0:1, :], in1=brow[0], op=ALU.add), r=[bpa, bbrow[0]], w=[bmrow[0], brstd])

            def tr(e):
                for i in range(4):
                    ins = e.matmul(pb[7][:, i:i + 1], lhsT=bm_t[0:1, 512 + i * 128:512 + (i + 1) * 128], rhs=one11[0:1, 0:1], start=True, stop=True)
                return ins
            PE(tr, r=[bmrow[0], bconst, brstd], w=[bP[7]])
            V(lambda e: e.tensor_copy(out=modT[:, 4 * j:4 * j + 4], in_=pb[7][:, 0:4]), r=[bP[7]], w=[bmod])

        NADA0 = 16
        ada_load(0, 0)
        for j in range(NADA0):
            if j + 1 < NADA0:
                ada_load(j + 1, (j + 1) % 2)
            ada_comp(j, j % 2)
        V(lambda e: e.tensor_scalar(out=modT[:, 32:64], in0=modT[:, 32:64], scalar1=1.0, scalar2=None, op0=ALU.add), r=[bmod], w=[bmod])
        SH1, SC1, G1, SH2, SC2, G2 = 0, 32, 64, 96, 128, 160

        def dump(ap_sb, ncols, rbufs):
            DS(lambda e: e.dma_start(out=dbgo[:, 0:ncols], in_=ap_sb), r=rbufs, w=[bdbg], sem="dbg")

        bdbg = Buf("dbg")
        bout = Buf("out")

        def finish():
            P.op("sync", None, reads=[bdbg, bout])
            P.emit()

        if stop == 0:
            dump(modT[:, :], 192, [bmod])
            finish()
            return nc

        def rstd_from(ps_ap, out_ap, n, inv_n, rb, wb_):
            V(lambda e: e.tensor_scalar(out=out_ap, in0=ps_ap, scalar1=inv_n, scalar2=EPS, op0=ALU.mult, op1=ALU.add), r=rb, w=wb_)
            A(lambda e: e.activation(out=out_ap, in_=out_ap, func=AF.Sqrt), r=wb_, w=wb_)
            V(lambda e: e.reciprocal(out=out_ap, in_=out_ap), r=wb_, w=wb_)

        Hv = H[:, :].rearrange("p (c t) -> p c t", c=NCH)
        Xv = X[:, :].rearrange("p (c t) -> p c t", c=NCH)
        Qsq = Q[:, 0:8192].rearrange("p (c t) -> p c t", c=NCH)

        def make_hT(tok0):
            for blk in range(4):
                t0 = tok0 + blk * 256
                DS(lambda e: e.dma_start(out=Xv, in_=xT[:, :, t0:t0 + 256]), w=[bX], sem="X")
                A(lambda e: e.activation(out=Q[:, 0:8192], in_=X[:, :], func=AF.Square), r=[bX], w=[bQ])

                def mm(e):
                    for c in range(NCH):
                        ins = e.matmul(pb[4][:, 0:256], lhsT=ones_bf[:, :], rhs=Qsq[:, c, :], start=(c == 0), stop=(c == NCH - 1))
                    return ins
                PE(mm, r=[bQ, bconst], w=[bP[4]])
                rs = rstd[:, 0:256]
                rstd_from(pb[4][:, 0:256], rs, 256, 1.0 / D, [bP[4]], [brstd])
                V(lambda e: e.tensor_tensor(out=Xv, in0=Xv, in1=rs.unsqueeze(1).to_broadcast([128, NCH, 256]), op=ALU.mult), r=[bX, brstd], w=[bX])
                lo = blk * 256

                def modu(e):
                    for c in range(NCH):
                        ins = e.tensor_scalar(out=Hv[:, c, lo:lo + 256], in0=Xv[:, c, :], scalar1=modT[:, SC1 + c:SC1 + c + 1], scalar2=modT[:, SH1 + c:SH1 + c + 1], op0=ALU.mult, op1=ALU.add)
                    return ins
                V(modu, r=[bX, bmod], w=[bH])

        gemm_k = [0]

        def gemm(jobs, act_v, act_bufs, segs, consume, extra_load=None):
            n = len(jobs)

            def load(i):
                k = gemm_k[0] + i
                s = k % 2
                src = jobs[i][0]
                DG(lambda e: e.dma_start(out=W[s][:, :], in_=src, max_dma_last_dim=8192), w=[bW[s]], sem=f"W{s}")
                if extra_load is not None:
                    extra_load(i)

            def comp(i):
                k = gemm_k[0] + i
                s = k % 2
                banks = [2 * (k % 2) + q for q in range(len(segs))]

                def mm(e):
                    for c in range(NCH):
                        for q, (lo, nn) in enumerate(segs):
                            ins = e.matmul(pb[banks[q]][:, 0:nn], lhsT=W[s][:, c * 128:(c + 1) * 128], rhs=act_v[:, c, lo:lo + nn], start=(c == 0), stop=(c == NCH - 1))
                    return ins
                PE(mm, r=[bW[s]] + act_bufs, w=[bP[b_] for b_ in banks])
                consume(i, jobs[i][1], banks)

            depth = 1
            for i in range(n + depth):
                if i < n:
                    load(i)
                if i - depth >= 0:
                    comp(i - depth)
            gemm_k[0] += n

        evk = [0]

        def evac_to_dram(dst_fn, segs):
            def consume(i, tag, banks):
                k = evk[0]
                evk[0] += 1
                s = k % 3
                off = 0
                for q, (lo, nn) in enumerate(segs):
                    o0 = off
                    if (k + q) % 2 == 0:
                        A(lambda e: e.activation(out=S[s][:, o0:o0 + nn], in_=pb[banks[q]][:, 0:nn], func=AF.Copy), r=[bP[banks[q]]], w=[bS[s]])
                    else:
                        V(lambda e: e.tensor_copy(out=S[s][:, o0:o0 + nn], in_=pb[banks[q]][:, 0:nn]), r=[bP[banks[q]]], w=[bS[s]])
                    off += nn
                tot = off
                DS(lambda e: e.dma_start(out=dst_fn(tag), in_=S[s][:, 0:tot]), r=[bS[s]], w=[bprojd], sem=f"S{s}")
            return consume

        bprojd = Buf("projd")
        FULL = [(0, 512), (512, 512)]

        make_hT(0)
        pre_tiles = list(range(8, 32)) + [48]
        gemm([(w_in[j], j) for j in pre_tiles], Hv, [bH], FULL, evac_to_dram(lambda j: projp[j], FULL))
        seg128 = [(896, 128)]
        gemm([(w_in[j], j) for j in range(65, 69)], Hv, [bH], seg128, evac_to_dram(lambda j: projp[j][:, 0:128], seg128))
        make_hT(1024)
        gemm([(w_in[j], j) for j in range(69)], Hv, [bH], FULL, evac_to_dram(lambda j: projo[j], FULL))

        if stop == 1:
            DS(lambda e: e.dma_start(out=S[0][:, :], in_=projo[3]), r=[bprojd], w=[bS[0]], sem="S0")
            V(lambda e: e.tensor_copy(out=FS[0][:, :], in_=S[0][:, :]), r=[bS[0]], w=[bFS[0]])
            dump(FS[0][:, :], 1024, [bFS[0]])
            finish()
            return nc

        Qv = Q[:, :].rearrange("p (a t) -> p a t", t=T)
        gqT = Qv[:, 0:8, :]
        gkT = Qv[:, 8:16, :]
        gvT = Hv[:, 0:16, :]
        ggT = Hv[:, 16:32, :]
        ga_s = S[2][0:16, :]
        bga = bS[2]
        DG(lambda e: e.dma_start(out=wg_b, in_=wg), w=[bwg, bFS[0], bFS[1]], sem="c1")
        DG(lambda e: e.dma_start(out=bg_b, in_=bg), w=[bwg, bFS[0], bFS[1]], sem="c1")
        qo = 16384
        qtl = Q[:, qo:qo + 1024].rearrange("p (a t) -> p a t", a=8)
        ktz = Q[:, qo + 1024:qo + 3072].rearrange("p (a b t) -> p a b t", a=8, b=2)
        k2T = Q[:, qo + 3072:qo + 4096].rearrange("p (a t) -> p a t", a=8)
        k2s = Q[:, qo + 4096:qo + 5120]
        v_sb = Q[:, qo + 5120:qo + 7168]
        attn_m = Q[:, qo + 7168:qo + 9216].rearrange("p (a t) -> p a t", a=16)
        o_n = attn_m
        mixst = Q[:, qo + 9216:qo + 11264].rearrange("p (a t) -> p a t", a=16)
        bqt, bkt, bk2T, bk2s, bvs, battn, bmix = [Buf(n) for n in "qt kt k2T k2s vs attn mix".split()]
        bon = battn
        e1 = X[:, 0:1024]
        ebx = X[:, 1024:2048]
        enb = X[:, 2048:3072]
        ek2 = X[:, 3072:4096]
        sqx = X[:, 4096:6144]
        be1, beb, benb, bek2, bsq = [Buf(n) for n in "e1 eb enb ek2 sq".split()]
        blast = small[:, 0:8]
        eblast = small[:, 8:16]
        ss16 = small[:, 16:32]
        maskc = masks_f[:, 0, :]
        bmixd = Buf("mixd")

        V(lambda e: e.memset(state[:, :, :], 0.0), w=[bstate])
        V(lambda e: e.memset(state_b[:, :, :, :], 0.0), w=[bstate_b])
        V(lambda e: e.memset(ktz, 0.0), w=[bkt])

        class _Stop(Exception):
            pass
        ckn = [0]

        def ck(bufs):
            ckn[0] += 1
            if stop == 20 + ckn[0]:
                P.op("sync", None, reads=bufs)
                raise _Stop()

        def gla_pass(proj, own):
            DSsplit(lambda a, b: gkT[:, a:b, :], lambda a, b: proj[8 + a:8 + b].rearrange("a p t -> p a t"), 8, 4, [bprojd], [bQ], "Q")
            DSsplit(lambda a, b: gvT[:, a:b, :], lambda a, b: proj[16 + a:16 + b].rearrange("a p t -> p a t"), 16, 4, [bprojd], [bH], "H")
            DS(lambda e: e.dma_start(out=ga_s, in_=proj[48][0:16, :]), r=[bprojd], w=[bga], sem="ga")
            if own:
                DSsplit(lambda a, b: gqT[:, a:b, :], lambda a, b: proj[a:b].rearrange("a p t -> p a t"), 8, 4, [bprojd], [bQ], "Q")
                DSsplit(lambda a, b: ggT[:, a:b, :], lambda a, b: proj[32 + a:32 + b].rearrange("a p t -> p a t"), 16, 4, [bprojd], [bH], "H")
                A(lambda e: e.activation(out=H[:, 16384:32768], in_=H[:, 16384:32768], func=AF.Silu), r=[bH], w=[bH])
                V(lambda e: e.tensor_scalar(out=H[:, 16384:32768], in0=H[:, 16384:32768], scalar1=gain_s[:, 0:1], scalar2=None, op0=ALU.mult), r=[bH, bconst], w=[bH])
            ck([bQ, bH, bga])
            for c in range(8):
                cs_ = slice(c * 128, (c + 1) * 128)

                def zmm(e):
                    for nb in range(2):
                        e.matmul(pb[nb][:, :], lhsT=ga_s[0:16, cs_], rhs=wg_b[0:16, nb * 512:(nb + 1) * 512], start=True, stop=False)
                        ins = e.matmul(pb[nb][:, :], lhsT=ones_bf[0:1, 0:128], rhs=bg_b[0:1, nb * 512:(nb + 1) * 512], start=False, stop=True)
                    return ins
                PE(zmm, r=[bga, bconst, bwg], w=[bP[0], bP[1]])
                ck([bP[0], bP[1]])
                for nb in range(2):
                    A(lambda e, nb=nb: e.activation(out=e1[:, nb * 512:(nb + 1) * 512], in_=pb[nb][:, :], func=AF.Exp, scale=-1.0), r=[bP[nb]], w=[be1])
                A(lambda e: e.activation(out=e1, in_=e1, func=AF.Ln, bias=1.0), r=[be1], w=[be1])

                def bmm(e):
                    for p in range(8):
                        ins = e.matmul(pb[2 + p // 4][:, (p % 4) * 128:(p % 4 + 1) * 128], lhsT=e1[:, p * 128:(p + 1) * 128], rhs=ltri[:, :], start=True, stop=True)
                    return ins
                ck([be1])
                PE(bmm, r=[be1, bconst], w=[bP[2], bP[3]])
                ck([bP[2], bP[3]])
                for k in range(2):
                    V(lambda e, k=k: e.tensor_copy(out=blast[:, 4 * k:4 * k + 4], in_=pb[2 + k][:, 127::128]), r=[bP[2 + k]], w=[bsmall])
                for k in range(2):
                    if own:
                        A(lambda e, k=k: e.activation(out=ebx[:, k * 512:(k + 1) * 512], in_=pb[2 + k][:, :], func=AF.Exp), r=[bP[2 + k]], w=[beb])
                        A(lambda e, k=k: e.activation(out=enb[:, k * 512:(k + 1) * 512], in_=pb[2 + k][:, :], func=AF.Exp, scale=-1.0), r=[bP[2 + k]], w=[benb])
                for p in range(8):
                    A(lambda e, p=p: e.activation(out=ek2[:, p * 128:(p + 1) * 128], in_=pb[2 + p // 4][:, (p % 4) * 128:(p % 4 + 1) * 128], func=AF.Exp, scale=-1.0, bias=blast[:, p:p + 1]), r=[bP[2 + p // 4], bsmall], w=[bek2])
                A(lambda e: e.activation(out=eblast, in_=blast, func=AF.Exp), r=[bsmall], w=[bsmall])
                ck([bek2, bsmall, beb, benb])
                v3 = lambda ap: ap.rearrange("p (a t) -> p a t", a=8)
                if own:
                    V(lambda e: e.scalar_tensor_tensor(out=qtl, in0=gqT[:, :, cs_], scalar=0.125, in1=v3(ebx), op0=ALU.mult, op1=ALU.mult), r=[bQ, beb], w=[bqt])
                    for hh in range(2):
                        V(lambda e, hh=hh: e.tensor_tensor(out=ktz[hh * 64:hh * 64 + 64, :, hh, :], in0=gkT[hh * 64:hh * 64 + 64, :, cs_], in1=v3(enb)[hh * 64:hh * 64 + 64, :, :], op=ALU.mult), r=[bQ, benb], w=[bkt])
                V(lambda e: e.tensor_tensor(out=k2T, in0=gkT[:, :, cs_], in1=v3(ek2), op=ALU.mult), r=[bQ, bek2], w=[bk2T])
                pbK = pb[4][:, :].bitcast(BF16)
                pbV = [pb[5][:, :].bitcast(BF16), pb[6][:, :].bitcast(BF16)]

                def trk(e):
                    for p in range(8):
                        ins = e.transpose(out=pbK[:, p * 128:(p + 1) * 128], in_=k2T[:, p, :], identity=ident_b[:, :])
                    return ins
                ck([bk2T, bqt, bkt])
                PE(trk, r=[bk2T, bconst], w=[bP[4]])
                ck([bP[4]])

                def trv(e):
                    for h in range(16):
                        ins = e.transpose(out=pbV[h // 8][:, (h % 8) * 128:(h % 8 + 1) * 128], in_=gvT[:, h, cs_], identity=ident_b[:, :])
                    return ins
                PE(trv, r=[bH, bconst], w=[bP[5], bP[6]])
                ck([bP[5], bP[6]])
                A(lambda e: e.activation(out=k2s, in_=pbK, func=AF.Copy), r=[bP[4]], w=[bk2s])
                V(lambda e: e.tensor_copy(out=v_sb[:, 0:1024], in_=pbV[0]), r=[bP[5]], w=[bvs])
                A(lambda e: e.activation(out=v_sb[:, 1024:2048], in_=pbV[1], func=AF.Copy), r=[bP[6]], w=[bvs])
                if own:
                    def amm(e):
                        for h in range(16):
                            p, r0 = h // 2, (h % 2) * 64
                            ins = e.matmul(pb[2 * (h % 2) + p // 4][:, (p % 4) * 128:(p % 4 + 1) * 128], lhsT=ktz[:, p, h % 2, :], rhs=qtl[:, p, :], start=True, stop=True)
                        return ins
                    PE(amm, r=[bkt, bqt], w=[bP[0], bP[1], bP[2], bP[3]])
                    attn_v = attn_m.rearrange("p (a b) t -> p a b t", b=2)
                    for k in range(4):
                        V(lambda e, k=k: e.tensor_tensor(out=attn_v[:, 4 * (k % 2):4 * (k % 2) + 4, k // 2, :], in0=pb[k][:, :].rearrange("p (a t) -> p a t", a=4), in1=maskc.unsqueeze(1).to_broadcast([128, 4, 128]), op=ALU.mult), r=[bP[k], bconst], w=[battn])

                    def omm(e):
                        for h in range(16):
                            p, r0 = h // 2, (h % 2) * 64
                            o_ap = pb[4 + h // 4][:, (h % 4) * 128:(h % 4 + 1) * 128]
                            e.matmul(o_ap, lhsT=attn_m[:, h, :], rhs=v_sb[:, h * 128:(h + 1) * 128], start=True, stop=False)
                            ins = e.matmul(o_ap, lhsT=qtl[:, p, :], rhs=state_b[:, p, h % 2, :], start=False, stop=True)
                        return ins
                    PE(omm, r=[battn, bvs, bqt, bstate_b], w=[bP[4], bP[5], bP[6], bP[7]])

                def smm(e):
                    for p in range(8):
                        ins = e.matmul(pb[p // 2][:, (p % 2) * 256:(p % 2 + 1) * 256], lhsT=k2s[:, p * 128:(p + 1) * 128], rhs=v_sb[:, p * 256:(p + 1) * 256], start=True, stop=True)
                    return ins
                ck([bk2s, bvs])
                PE(smm, r=[bk2s, bvs], w=[bP[0], bP[1], bP[2], bP[3]])
                ck([bP[0], bP[1], bP[2], bP[3]])

                def supd(e):
                    for p in range(8):
                        for hh in range(2):
                            r0 = hh * 64
                            ins = e.scalar_tensor_tensor(out=state[r0:r0 + 64, p, :], in0=state[r0:r0 + 64, p, :], scalar=eblast[r0:r0 + 64, p:p + 1], in1=pb[p // 2][r0:r0 + 64, (p % 2) * 256 + hh * 128:(p % 2) * 256 + hh * 128 + 128], op0=ALU.mult, op1=ALU.add)
                    return ins
                V(supd, r=[bstate, bstate_b, bsmall, bP[0], bP[1], bP[2], bP[3]], w=[bstate])
                if (not own) and c == 7:
                    V(lambda e: e.tensor_scalar(out=state[:, :, :], in0=state[:, :, :], scalar1=flag_s[:, 0:1], scalar2=None, op0=ALU.mult), r=[bstate, bconst], w=[bstate])
                for hh in range(2):
                    V(lambda e, hh=hh: e.tensor_copy(out=state_b[hh * 64:hh * 64 + 64, :, hh, :], in_=state[hh * 64:hh * 64 + 64, :, :]), r=[bstate], w=[bstate_b])
                if own:
                    for k in range(4):
                        A(lambda e, k=k: e.activation(out=sqx[:, k * 512:(k + 1) * 512], in_=pb[4 + k][:, :], func=AF.Square), r=[bP[4 + k]], w=[bsq])
                    V(lambda e: e.tensor_reduce(out=ss16, in_=sqx.rearrange("p (a t) -> p a t", a=16), axis=AX.X, op=ALU.add), r=[bsq], w=[bsmall])
                    rstd_from(ss16, ss16, 16, 1.0 / 128, [bsmall], [bsmall])
                    for k in range(4):
                        V(lambda e, k=k: e.tensor_tensor(out=o_n[:, 4 * k:4 * k + 4, :], in0=pb[4 + k][:, :].rearrange("p (a t) -> p a t", a=4), in1=ss16[:, 4 * k:4 * k + 4].unsqueeze(2).to_broadcast([128, 4, 128]), op=ALU.mult), r=[bP[4 + k], bsmall], w=[bon])
                    pbT = [pb[4][:, :].bitcast(BF16), pb[5][:, :].bitcast(BF16)]

                    def tro(e):
                        for h in range(16):
                            ins = e.transpose(out=pbT[h // 8][:, (h % 8) * 128:(h % 8 + 1) * 128], in_=o_n[:, h, :], identity=ident_b[:, :])
                        return ins
                    PE(tro, r=[bon, bconst], w=[bP[4], bP[5]])
                    for k in range(2):
                        V(lambda e, k=k: e.tensor_tensor(out=mixst[:, 8 * k:8 * k + 8, :], in0=pbT[k].rearrange("p (a t) -> p a t", a=8), in1=ggT[:, 8 * k:8 * k + 8, cs_], op=ALU.mult), r=[bP[4 + k], bH], w=[bmix])
                    DSsplit(lambda a, b: mixd[a:b, :, cs_].rearrange("a p t -> p a t"), lambda a, b: mixst[:, a:b, :], 16, 4, [bmix], [bmixd], "mix")

        try:
            gla_pass(projp, False)
            ck([bstate, bstate_b])
            gla_pass(projo, True)
        except _Stop:
            finish()
            return nc

        if stop == 2:
            DS(lambda e: e.dma_start(out=S[0][:, :], in_=mixd[5]), r=[bmixd], w=[bS[0]], sem="S0")
            V(lambda e: e.tensor_copy(out=FS[0][:, :], in_=S[0][:, :]), r=[bS[0]], w=[bFS[0]])
            dump(FS[0][:, :], 1024, [bFS[0]])
            finish()
            return nc

        sqT = Hv[:, 0:16, :]
        TW = 1152
        skT = Q[:, 0:2 * TW].rearrange("p (a t) -> p a t", a=2)
        svT = Q[:, 2304:2304 + 2 * TW].rearrange("p (a t) -> p a t", a=2)
        so = 4608
        kdT = Q[:, so:so + 9 * 1024].rearrange("p (n g b t) -> p n g b t", n=9, g=4, b=2)
        so += 9 * 1024
        vaug = Q[:, so:so + 9 * 260].rearrange("p (n g d) -> p n g d", n=9, g=4)
        so += 9 * 260
        so = (so + 63) // 64 * 64
        qr_b = Q[:, so:so + 2048]
        so += 2048
        qTb = Q[:, so:so + 2048].rearrange("p (a t) -> p a t", a=16)
        so += 2048
        pexp = Q[:, so:so + 2048].rearrange("p (a t) -> p a t", a=16)
        so += 2048
        kdup = Q[:, so:so + 512].rearrange("p (g d) -> p g d", g=4)
        so += 512
        osn_b = Q[:, so:so + 2048]
        so += 2048
        mix2 = Q[:, so:so + 2048].rearrange("p (a t) -> p a t", a=16)
        so += 2048
        bkd, bva, bqr, bqT, bpex, bkdup, bosn, bmix2 = [Buf(n) for n in "kd va qr qT pex kdup osn mix2".split()]
        qt_f = X[:, 0:2048]
        tmp_f = X[:, 2048:4096]
        osw = X[:, 4096:6144]
        t2_f = X[:, 6144:8192]
        bqf, btf, bosw, bt2 = [Buf(n) for n in "qf tf osw t2".split()]
        ss32 = small[:, 32:64]
        den8 = small[:, 64:72]

        DSsplit(lambda a, b: sqT[:, a:b, :], lambda a, b: projo[49 + a:49 + b].rearrange("a p t -> p a t"), 16, 4, [bprojd], [bH], "H")
        DS(lambda e: e.dma_start(out=skT[:, :, 128:TW], in_=projo[65:67].rearrange("a p t -> p a t")), r=[bprojd], w=[bQ], sem="Q")
        DS(lambda e: e.dma_start(out=svT[:, :, 128:TW], in_=projo[67:69].rearrange("a p t -> p a t")), r=[bprojd], w=[bQ], sem="Q")
        DS(lambda e: e.dma_start(out=skT[:, :, 0:128], in_=projp[65:67, :, 0:128].rearrange("a p t -> p a t")), r=[bprojd], w=[bQ], sem="Q")
        DS(lambda e: e.dma_start(out=svT[:, :, 0:128], in_=projp[67:69, :, 0:128].rearrange("a p t -> p a t")), r=[bprojd], w=[bQ], sem="Q")
        V(lambda e: e.memset(vaug[:, :, :, 64:65], 1.0), w=[bva])
        V(lambda e: e.memset(Q[:, 4608:4608 + 9 * 1024], 0.0), r=[bQ], w=[bkd])

        def rope(xv, nh, blk, rb):
            cosb = cs_s[:, blk, 0, :].unsqueeze(1).to_broadcast([128, nh, 32])
            sinb = cs_s[:, blk, 1, :].unsqueeze(1).to_broadcast([128, nh, 32])
            x1 = xv[:, :, 0:32]
            x2 = xv[:, :, 32:64]
            ta = tmp_f[:, 0:nh * 32].rearrange("p (h d) -> p h d", h=nh)
            tb = tmp_f[:, 1024:1024 + nh * 32].rearrange("p (h d) -> p h d", h=nh)
            tc_ = t2_f[:, 0:nh * 32].rearrange("p (h d) -> p h d", h=nh)
            td = t2_f[:, 1024:1024 + nh * 32].rearrange("p (h d) -> p h d", h=nh)
            V(lambda e: e.tensor_tensor(out=ta, in0=x1, in1=cosb, op=ALU.mult), r=rb + [bconst], w=[btf])
            V(lambda e: e.tensor_tensor(out=tb, in0=x2, in1=sinb, op=ALU.mult), r=rb + [bconst], w=[btf])
            V(lambda e: e.tensor_tensor(out=tc_, in0=x2, in1=cosb, op=ALU.mult), r=rb + [bconst], w=[bt2])
            V(lambda e: e.tensor_tensor(out=td, in0=x1, in1=sinb, op=ALU.mult), r=rb + [bconst], w=[bt2])
            V(lambda e: e.tensor_tensor(out=x1, in0=ta, in1=tb, op=ALU.subtract), r=[btf], w=rb)
            V(lambda e: e.tensor_tensor(out=x2, in0=tc_, in1=td, op=ALU.add), r=[bt2], w=rb)

        def normrope(xf, nh, gain_b, blk, rb):
            xv = xf.rearrange("p (h d) -> p h d", h=nh)
            sq_ = tmp_f[:, 0:nh * 64]
            A(lambda e: e.activation(out=sq_, in_=xf, func=AF.Square), r=rb, w=[btf])
            ssx = ss32[:, 0:nh]
            V(lambda e: e.tensor_reduce(out=ssx, in_=sq_.rearrange("p (h d) -> p h d", h=nh), axis=AX.X, op=ALU.add), r=[btf], w=[bsmall])
            rstd_from(ssx, ssx, nh, 1.0 / 64, [bsmall], [bsmall])
            V(lambda e: e.tensor_tensor(out=xv, in0=xv, in1=ssx.unsqueeze(2).to_broadcast([128, nh, 64]), op=ALU.mult), r=rb + [bsmall], w=rb)
            if gain_b is not None:
                V(lambda e: e.tensor_tensor(out=xv, in0=xv, in1=gain_b.unsqueeze(1).to_broadcast([128, nh, 64]), op=ALU.mult), r=rb + [bconst], w=rb)
            if blk is not None:
                rope(xv, nh, blk, rb)

        ck2n = [0]

        def ck2(bufs):
            ck2n[0] += 1
            if stop == 200 + ck2n[0]:
                P.op("sync", None, reads=bufs)
                finish()
                raise StopBuild(nc)

        ck2([bH, bQ, bva, bkd])
        for n in range(9):
            ts_ = slice(n * 128, (n + 1) * 128)
            pbX = pb[0][:, :].bitcast(BF16)

            def trkv(e):
                for a in range(2):
                    e.transpose(out=pbX[:, a * 128:(a + 1) * 128], in_=skT[:, a, ts_], identity=ident_b[:, :])
                for a in range(2):
                    ins = e.transpose(out=pbX[:, 256 + a * 128:256 + (a + 1) * 128], in_=svT[:, a, ts_], identity=ident_b[:, :])
                return ins
            PE(trkv, r=[bQ, bconst], w=[bP[0]])
            ck2([bP[0]])
            kf = qt_f[:, 0:256]
            V(lambda e: e.tensor_copy(out=kf, in_=pbX[:, 0:256]), r=[bP[0]], w=[bqf])
            V(lambda e: e.tensor_copy(out=vaug[:, n, :, 0:64], in_=pbX[:, 256:512].rearrange("p (g d) -> p g d", g=4)), r=[bP[0]], w=[bva])
            ck2([bqf, bva])
            normrope(kf, 4, kn_b[:, :], n, [bqf])
            ck2([bqf, btf, bt2, bsmall])
            kfv = kf.rearrange("p (g d) -> p g d", g=4)
            V(lambda e: e.tensor_copy(out=kdup[:, :, 0:64], in_=kfv), r=[bqf], w=[bkdup])
            V(lambda e: e.tensor_copy(out=kdup[:, :, 64:128], in_=kfv), r=[bqf], w=[bkdup])
            pbY = pb[1][:, :].bitcast(BF16)

            def trkd(e):
                for g in range(4):
                    ins = e.transpose(out=pbY[:, g * 128:(g + 1) * 128], in_=kdup[:, g, :], identity=ident_b[:, :])
                return ins
            PE(trkd, r=[bkdup, bconst], w=[bP[1]])
            for hh in range(2):
                V(lambda e, hh=hh: e.tensor_copy(out=kdT[hh * 64:hh * 64 + 64, n, :, hh, :], in_=pbY[hh * 64:hh * 64 + 64, 0:512].rearrange("p (g t) -> p g t", g=4)), r=[bP[1]], w=[bkd])
            ck2([bkd, bkdup])
            if n == 0:
                continue
            j = n - 1
            to_ = slice(j * 128, (j + 1) * 128)
            pbQ2 = [pb[2][:, :].bitcast(BF16), pb[3][:, :].bitcast(BF16)]

            def trq(e):
                for a in range(16):
                    ins = e.transpose(out=pbQ2[a // 8][:, (a % 8) * 128:(a % 8 + 1) * 128], in_=sqT[:, a, to_], identity=ident_b[:, :])
                return ins
            PE(trq, r=[bH, bconst], w=[bP[2], bP[3]])
            V(lambda e: e.tensor_copy(out=qt_f[:, 0:1024], in_=pbQ2[0]), r=[bP[2]], w=[bqf])
            A(lambda e: e.activation(out=qt_f[:, 1024:2048], in_=pbQ2[1], func=AF.Copy), r=[bP[3]], w=[bqf])
            normrope(qt_f, 32, qn_b[:, :], n, [bqf])
            V(lambda e: e.tensor_copy(out=qr_b, in_=qt_f), r=[bqf], w=[bqr])

            def trqb(e):
                for a in range(16):
                    ins = e.transpose(out=pbQ2[a // 8][:, (a % 8) * 128:(a % 8 + 1) * 128], in_=qr_b[:, a * 128:(a + 1) * 128], identity=ident_b[:, :])
                return ins
            PE(trqb, r=[bqr, bconst], w=[bP[2], bP[3]])
            V(lambda e: e.tensor_copy(out=qTb[:, 0:8, :], in_=pbQ2[0].rearrange("p (a t) -> p a t", a=8)), r=[bP[2]], w=[bqT])
            A(lambda e: e.activation(out=qTb[:, 8:16, :], in_=pbQ2[1].rearrange("p (a t) -> p a t", a=8), func=AF.Copy), r=[bP[3]], w=[bqT])
            ck2([bqT, bqr, bqf])
            mprev = masks_f[:, 2, :] if j == 0 else masks_f[:, 1, :]
            for rd in range(4):
                ja = NADA0 + (n - 1) * 4 + rd
                if ja == NADA0:
                    ada_load(ja, 1, late=True)
                ada_comp(ja, 1, late=True)
                if ja + 1 < 48:
                    ada_load(ja + 1, 1, late=True)
                def smm2(e):
                    for hh in range(8):
                        h = 8 * rd + hh
                        p, r0 = h // 2, (h % 2) * 64
                        par, q_ = hh % 2, hh // 2
                        for kb in range(2):
                            slot = q_ * 2 + kb
                            ins = e.matmul(pb[4 + 2 * par + slot // 4][:, (slot % 4) * 128:(slot % 4 + 1) * 128], lhsT=kdT[:, n - 1 + kb, rd, h % 2, :], rhs=qTb[:, p, :], start=True, stop=True)
                    return ins
                PE(smm2, r=[bkd, bqT], w=[bP[4], bP[5], bP[6], bP[7]])
                ck2([bP[4], bP[5], bP[6], bP[7]])
                pe5 = pexp.rearrange("p (q r k) t -> p q r k t", r=2, k=2)
                for k in range(4):
                    A(lambda e, k=k: e.activation(out=pe5[:, 2 * (k % 2):2 * (k % 2) + 2, k // 2, :, :], in_=pb[4 + k][:, :].rearrange("p (q k t) -> p q k t", q=2, k=2), func=AF.Exp, scale=0.125), r=[bP[4 + k]], w=[bpex])
                pe4 = pexp.rearrange("p (h k) t -> p h k t", k=2)
                V(lambda e: e.tensor_tensor(out=pe4[:, :, 0, :], in0=pe4[:, :, 0, :], in1=mprev.unsqueeze(1).to_broadcast([128, 8, 128]), op=ALU.mult), r=[bpex, bconst], w=[bpex])
                V(lambda e: e.tensor_tensor(out=pe4[:, :, 1, :], in0=pe4[:, :, 1, :], in1=maskc.unsqueeze(1).to_broadcast([128, 8, 128]), op=ALU.mult), r=[bpex, bconst], w=[bpex])

                ck2([bpex])

                def pvmm(e):
                    for hh in range(8):
                        o_ap = pb[hh // 4][:, (hh % 4) * 65:(hh % 4) * 65 + 65]
                        for kb in range(2):
                            ins = e.matmul(o_ap, lhsT=pexp[:, hh * 2 + kb, :], rhs=vaug[:, n - 1 + kb, rd, :], start=(kb == 0), stop=(kb == 1))
                    return ins
                PE(pvmm, r=[bpex, bva], w=[bP[0], bP[1]])
                ck2([bP[0], bP[1]])
                for k in range(2):
                    ov = pb[k][:, 0:260].rearrange("p (h d) -> p h d", h=4)
                    dn = den8[:, 4 * k:4 * k + 4]
                    h0 = 8 * rd + 4 * k
                    V(lambda e, ov=ov, dn=dn, h0=h0: e.tensor_tensor(out=dn.unsqueeze(2), in0=ov[:, :, 64:65], in1=esink[:, h0:h0 + 4].unsqueeze(2), op=ALU.add), r=[bP[k], bconst], w=[bsmall])
                    V(lambda e, dn=dn: e.reciprocal(out=dn, in_=dn), r=[bsmall], w=[bsmall])
                    V(lambda e, ov=ov, dn=dn, h0=h0: e.tensor_tensor(out=osw[:, h0 * 64:(h0 + 4) * 64].rearrange("p (h d) -> p h d", h=4), in0=ov[:, :, 0:64], in1=dn.unsqueeze(2).to_broadcast([128, 4, 64]), op=ALU.mult), r=[bP[k], bsmall], w=[bosw])
            ck2([bosw, bsmall])
            normrope(osw, 32, None, None, [bosw])
            V(lambda e: e.tensor_copy(out=osn_b, in_=osw), r=[bosw], w=[bosn])

            def tro2(e):
                for a in range(16):
                    ins = e.transpose(out=pbQ2[a // 8][:, (a % 8) * 128:(a % 8 + 1) * 128], in_=osn_b[:, a * 128:(a + 1) * 128], identity=ident_b[:, :])
                return ins
            PE(tro2, r=[bosn, bconst], w=[bP[2], bP[3]])
            for k in range(2):
                V(lambda e, k=k: e.tensor_scalar(out=mix2[:, 8 * k:8 * k + 8, :], in0=pbQ2[k].rearrange("p (a t) -> p a t", a=8), scalar1=on_s[:, 0:1], scalar2=None, op0=ALU.mult), r=[bP[2 + k], bconst], w=[bmix2])
            DSsplit(lambda a, b: mixd[16 + a:16 + b, :, to_].rearrange("a p t -> p a t"), lambda a, b: mix2[:, a:b, :], 16, 4, [bmix2], [bmixd], "mix2")

        V(lambda e: e.tensor_scalar(out=modT[:, 128:160], in0=modT[:, 128:160], scalar1=1.0, scalar2=None, op0=ALU.add), r=[bmod], w=[bmod])
        ck2([bmixd])
        if stop == 3:
            for qi, src in enumerate([mixd[5], mixd[21], projo[3]]):
                DS(lambda e, src=src: e.dma_start(out=S[0][:, :], in_=src), r=[bmixd, bprojd], w=[bS[0]], sem="S0")
                V(lambda e: e.tensor_copy(out=FS[0], in_=S[0][:, :]), r=[bS[0]], w=[bFS[0]])
                DS(lambda e, qi=qi: e.dma_start(out=dbgo[:, qi * 1024:(qi + 1) * 1024], in_=FS[0]), r=[bFS[0]], w=[bdbg], sem="dbg")
            finish()
            return nc

        DSsplit(lambda a, b: Hv[:, a:b, :], lambda a, b: mixd[a:b].rearrange("a p t -> p a t"), 32, 4, [bmixd], [bH], "H")
        bx1d = Buf("x1d")
        sqb = Q[:, 0:1024]
        bsqb = Buf("sqb")

        def xo_load(i):
            s = i % 2
            DS(lambda e: e.dma_start(out=FS[s][:, :], in_=xT[:, i, 1024:2048]), w=[bFS[s]], sem=f"FS{s}")

        def op_consume(i, tag, banks):
            s = i % 2
            for q in range(2):
                V(lambda e, q=q: e.scalar_tensor_tensor(out=FS[s][:, q * 512:(q + 1) * 512], in0=pb[banks[q]][:, :], scalar=modT[:, G1 + i:G1 + i + 1], in1=FS[s][:, q * 512:(q + 1) * 512], op0=ALU.mult, op1=ALU.add), r=[bP[banks[q]], bmod, bFS[s]], w=[bFS[s]])
            A(lambda e: e.activation(out=sqb, in_=FS[s][:, :], func=AF.Square), r=[bFS[s]], w=[bsqb])

            def mm(e):
                for q in range(2):
                    ins = e.matmul(pb[6 + q][:, :], lhsT=ones_bf[:, :], rhs=sqb[:, q * 512:(q + 1) * 512], start=(i == 0), stop=(i == 31))
                return ins
            PE(mm, r=[bsqb, bconst], w=[bP[6], bP[7]])
            DS(lambda e: e.dma_start(out=x1d[i], in_=FS[s][:, :]), r=[bFS[s]], w=[bx1d], sem=f"FSo{s}")

        gemm([(w_out[j], j) for j in range(32)], Hv, [bH], FULL, op_consume, extra_load=xo_load)
        for q in range(2):
            rstd_from(pb[6 + q][:, :], rstd[:, q * 512:(q + 1) * 512], 512, 1.0 / D, [bP[6 + q]], [brstd])

        if stop == 4:
            DS(lambda e: e.dma_start(out=FS[0][:, :], in_=x1d[7]), r=[bx1d], w=[bFS[0]], sem="FS0")
            dump(FS[0][:, :], 1024, [bFS[0]])
            finish()
            return nc

        for i in range(32):
            s = i % 2
            DS(lambda e, i=i, s=s: e.dma_start(out=FS[s][:, :], in_=x1d[i]), r=[bx1d], w=[bFS[s]], sem=f"FS{s}")
            V(lambda e, s=s: e.tensor_tensor(out=FS[s][:, :], in0=FS[s][:, :], in1=rstd[:, :], op=ALU.mult), r=[bFS[s], brstd], w=[bFS[s]])
            V(lambda e, i=i, s=s: e.tensor_scalar(out=Hv[:, i, :], in0=FS[s][:, :], scalar1=modT[:, SC2 + i:SC2 + i + 1], scalar2=modT[:, SH2 + i:SH2 + i + 1], op0=ALU.mult, op1=ALU.add), r=[bFS[s], bmod], w=[bH])

        qTp = Q[:, 0:16384].rearrange("p (a t) -> p a t", a=16)
        bqTp = Buf("qTp")

        def q_consume(i, tag, banks):
            for q in range(2):
                if q == 0:
                    A(lambda e: e.activation(out=qTp[:, i, 0:512], in_=pb[banks[0]][:, :], func=AF.Copy), r=[bP[banks[0]]], w=[bqTp, bQ])
                else:
                    V(lambda e: e.tensor_copy(out=qTp[:, i, 512:1024], in_=pb[banks[1]][:, :]), r=[bP[banks[1]]], w=[bqTp, bQ])
        gemm([(w_q[j], j) for j in range(16)], Hv, [bH], FULL, q_consume)

        idxT = FSb[:, 0:3072].rearrange("p (f t) -> p f t", f=3)
        bidxT = Buf("idxT")
        s_sb = X[:, 0:2048].rearrange("p (f h k) -> p f h k", f=2, h=8)
        tmp128 = X[:, 2048:2176]
        cand = X[:, 2304:4352].rearrange("p (h a b) -> p h a b", h=8, a=16)
        tmp256 = X[:, 4352:4608]
        eq = X[:, 4608:6656].rearrange("p (h a b) -> p h a b", h=8, a=16)
        tk = X[:, 6656:7680]
        tki = X[:, 7680:8192].bitcast(U32)
        btk = Buf("tk")
        v12 = tk[:, 0:256].rearrange("p (f h k) -> p f h k", f=2, h=8)
        i12f = tk[:, 256:512].rearrange("p (f h k) -> p f h k", f=2, h=8)
        tv = tk[:, 512:640].rearrange("p (h k) -> p h k", h=8)
        kk = tk[:, 640:896].rearrange("p (f h k) -> p f h k", f=2, h=8)
        sel = tk[:, 896:1024]
        i12u = tki[:, 0:256].rearrange("p (f h k) -> p f h k", f=2, h=8)
        tpos = tki[:, 256:384].rearrange("p (h k) -> p h k", h=8)
        kku = tki[:, 384:512].rearrange("p (h k) -> p h k", h=8)
        res3 = sb("res3", [128, 3, 128], F32)
        bres = Buf("res3")
        bss, bcand, beq = Buf("s_sb"), Buf("cand"), Buf("eq")
        iota16 = iota_f[:, 0:16]
        NEG = -1e30

        def top16(src, nsrc, tmp, vout, iout, rb):
            V(lambda e: e.max(out=vout[:, 0:8], in_=src), r=rb, w=[btk])
            V(lambda e: e.max_index(out=iout[:, 0:8], in_max=vout[:, 0:8], in_values=src), r=rb + [btk], w=[btk])
            V(lambda e: e.match_replace(out=tmp, in_to_replace=vout[:, 0:8], in_values=src, imm_value=NEG), r=rb + [btk], w=[bX])
            V(lambda e: e.max(out=vout[:, 8:16], in_=tmp), r=[bX], w=[btk])
            V(lambda e: e.max_index(out=iout[:, 8:16], in_max=vout[:, 8:16], in_values=tmp), r=[bX, btk], w=[btk])

        for tt in range(8):
            tsl = slice(tt * 128, (tt + 1) * 128)

            def smm3(e):
                for f in range(2):
                    for h in range(8):
                        ins = e.matmul(pb[f * 2 + h // 4][:, (h % 4) * 128:(h % 4 + 1) * 128], lhsT=qTp[:, h * 2 + f, tsl], rhs=keys_b[:, f, :], start=True, stop=True)
                return ins
            PE(smm3, r=[bqTp, bconst], w=[bP[0], bP[1], bP[2], bP[3]])
            for k in range(4):
                dst = X[:, k * 512:(k + 1) * 512]
                if k % 2 == 0:
                    V(lambda e, k=k, dst=dst: e.tensor_copy(out=dst, in_=pb[k][:, :]), r=[bP[k]], w=[bX])
                else:
                    A(lambda e, k=k, dst=dst: e.activation(out=dst, in_=pb[k][:, :], func=AF.Copy), r=[bP[k]], w=[bX])
            for f in range(2):
                for h in range(8):
                    top16(s_sb[:, f, h, :], 128, tmp128, v12[:, f, h, :], i12u[:, f, h, :], [bX])
            V(lambda e: e.tensor_copy(out=tk[:, 256:512], in_=tki[:, 0:256]), r=[btk], w=[btk])
            V(lambda e: e.tensor_tensor(out=cand, in0=v12[:, 0, :, :].unsqueeze(3).to_broadcast([128, 8, 16, 16]), in1=v12[:, 1, :, :].unsqueeze(2).to_broadcast([128, 8, 16, 16]), op=ALU.add), r=[btk], w=[bX])
            for h in range(8):
                top16(cand[:, h, :, :].rearrange("p a b -> p (a b)"), 256, tmp256, tv[:, h, :], tpos[:, h, :], [bX])
            gate = res3[:, 2, :].rearrange("p (h k) -> p h k", h=8)
            V(lambda e: e.tensor_tensor(out=gate, in0=tv, in1=tv[:, :, 0:1].to_broadcast([128, 8, 16]), op=ALU.subtract), r=[btk], w=[bres])
            A(lambda e: e.activation(out=gate, in_=gate, func=AF.Exp), r=[bres], w=[bres])
            gs8 = small[:, 80:88]
            V(lambda e: e.tensor_reduce(out=gs8, in_=gate, axis=AX.X, op=ALU.add), r=[bres], w=[bsmall])
            V(lambda e: e.reciprocal(out=gs8, in_=gs8), r=[bsmall], w=[bsmall])
            V(lambda e: e.tensor_tensor(out=gate, in0=gate, in1=gs8.unsqueeze(2).to_broadcast([128, 8, 16]), op=ALU.mult), r=[bres, bsmall], w=[bres])
            V(lambda e: e.tensor_single_scalar(out=kku, in_=tpos, scalar=4, op=ALU.logical_shift_right), r=[btk], w=[btk])
            V(lambda e: e.tensor_copy(out=kk[:, 0, :, :], in_=kku), r=[btk], w=[btk])
            V(lambda e: e.tensor_single_scalar(out=kku, in_=tpos, scalar=15, op=ALU.bitwise_and), r=[btk], w=[btk])
            V(lambda e: e.tensor_copy(out=kk[:, 1, :, :], in_=kku), r=[btk], w=[btk])
            for f in range(2):
                V(lambda e, f=f: e.tensor_tensor(out=eq, in0=kk[:, f, :, :].unsqueeze(3).to_broadcast([128, 8, 16, 16]), in1=iota16.unsqueeze(1).unsqueeze(1).to_broadcast([128, 8, 16, 16]), op=ALU.is_equal), r=[btk, bconst], w=[bX])
                V(lambda e, f=f: e.tensor_tensor(out=eq, in0=eq, in1=i12f[:, f, :, :].unsqueeze(2).to_broadcast([128, 8, 16, 16]), op=ALU.mult), r=[bX, btk], w=[bX])
                V(lambda e, f=f: e.tensor_reduce(out=res3[:, f, :].rearrange("p (h k) -> p h k", h=8), in_=eq, axis=AX.X, op=ALU.add), r=[bX], w=[bres])

            def tr3(e):
                for f in range(3):
                    ins = e.transpose(out=pb[4][:, f * 128:(f + 1) * 128], in_=res3[:, f, :], identity=ident_f[:, :])
                return ins
            PE(tr3, r=[bres, bconst], w=[bP[4]])
            V(lambda e: e.tensor_copy(out=idxT[:, :, tsl], in_=pb[4][:, 0:384].rearrange("p (f t) -> p f t", f=3)), r=[bP[4]], w=[bidxT, bFS[0], bFS[1]])

        if stop == 5:
            V(lambda e: e.tensor_copy(out=X[:, 0:3072], in_=FSb[:, 0:3072]), r=[bidxT], w=[bX])
            DS(lambda e: e.dma_start(out=dbgo[:, 0:3072], in_=X[:, 0:3072]), r=[bX], w=[bdbg], sem="dbg")
            for qi, src in enumerate([mixd[5], mixd[21]]):
                DS(lambda e, src=src: e.dma_start(out=S[0][:, :], in_=src), r=[bmixd, bprojd], w=[bS[0]], sem="S0")
                V(lambda e, qi=qi: e.tensor_copy(out=X[:, 4096 + qi * 1024:4096 + (qi + 1) * 1024], in_=S[0][:, :]), r=[bS[0]], w=[bX])
            DS(lambda e: e.dma_start(out=dbgo[:, 3072:5120], in_=X[:, 4096:6144]), r=[bX], w=[bdbg], sem="dbg")
            DS(lambda e: e.dma_start(out=X[:, 6144:7168], in_=x1d[7]), r=[bx1d], w=[bX], sem="X")
            DS(lambda e: e.dma_start(out=dbgo[:, 5120:6144], in_=X[:, 6144:7168]), r=[bX], w=[bdbg], sem="dbg")
            finish()
            return nc

        Gblk = Q[:, 0:16384].rearrange("p (i t) -> p i t", t=128)
        Aoh = [Q[:, 16384 + b_ * 4096:16384 + b_ * 4096 + 2048].rearrange("p (t i) -> p t i", i=128) for b_ in range(2)]
        Boh = [Q[:, 16384 + b_ * 4096 + 2048:16384 + b_ * 4096 + 4096].rearrange("p (t i) -> p t i", i=128) for b_ in range(2)]
        bG = Buf("Gblk")
        bA = [Buf("Aoh0"), Buf("Aoh1")]
        bB = [Buf("Boh0"), Buf("Boh1")]
        bGd = Buf("Gd")
        bGdq = [Buf(f"Gd{q}") for q in range(32)]
        iob = iota_f[:, :].unsqueeze(1).to_broadcast([128, 16, 128])
        for tb in range(8):
            for sub in range(8):
                b_ = sub % 2
                t0 = tb * 128 + sub * 16
                V(lambda e: e.tensor_tensor(out=Aoh[b_], in0=iob, in1=idxT[:, 0, t0:t0 + 16].unsqueeze(2).to_broadcast([128, 16, 128]), op=ALU.is_equal), r=[bidxT, bconst], w=[bA[b_]])
                V(lambda e: e.tensor_tensor(out=Boh[b_], in0=iob, in1=idxT[:, 1, t0:t0 + 16].unsqueeze(2).to_broadcast([128, 16, 128]), op=ALU.is_equal), r=[bidxT, bconst], w=[bB[b_]])
                V(lambda e: e.tensor_tensor(out=Boh[b_], in0=Boh[b_], in1=idxT[:, 2, t0:t0 + 16].unsqueeze(2).to_broadcast([128, 16, 128]), op=ALU.mult), r=[bidxT, bB[b_]], w=[bB[b_]])
                bks = [4 * b_ + k for k in range(4)]

                def gmm(e):
                    for t in range(16):
                        ins = e.matmul(pb[bks[t // 4]][:, (t % 4) * 128:(t % 4 + 1) * 128], lhsT=Aoh[b_][:, t, :], rhs=Boh[b_][:, t, :], start=True, stop=True)
                    return ins
                PE(gmm, r=[bA[b_], bB[b_]], w=[bP[k_] for k_ in bks])
                for k in range(4):
                    tl0 = sub * 16 + k * 4
                    dst = Gblk[:, :, tl0:tl0 + 4].rearrange("p i t -> p t i")
                    src = pb[bks[k]][:, :].rearrange("p (t i) -> p t i", t=4)
                    A(lambda e, dst=dst, src=src: e.activation(out=dst, in_=src, func=AF.Copy), r=[bP[bks[k]]], w=[bG])
            for q in range(32):
                DS(lambda e, q=q, tb=tb: e.dma_start(out=Gd[4 * q:4 * q + 4, :, tb * 128:(tb + 1) * 128].rearrange("j p t -> p j t"), in_=Gblk[:, 4 * q:4 * q + 4, :]), r=[bG], w=[bGdq[q]], sem=f"Gd{q}")

        if stop == 6:
            for qi, jj_ in enumerate([5, 77]):
                DS(lambda e, jj_=jj_: e.dma_start(out=S[0][:, :], in_=Gd[jj_]), r=bGdq, w=[bS[0]], sem="S0")
                V(lambda e, qi=qi: e.tensor_copy(out=X[:, qi * 1024:(qi + 1) * 1024], in_=S[0][:, :]), r=[bS[0]], w=[bX])
            V(lambda e: e.tensor_copy(out=X[:, 2048:5120], in_=FSb[:, 0:3072]), r=[bidxT], w=[bX])
            DS(lambda e: e.dma_start(out=dbgo[:, 0:5120], in_=X[:, 0:5120]), r=[bX], w=[bdbg], sem="dbg")
            finish()
            return nc

        bhTd = Buf("hTd")
        GS = [Q[:, 24576 + i * 1024:24576 + (i + 1) * 1024] for i in range(3)]
        bGS = [Buf(f"GS{i}") for i in range(3)]

        def g_load(i):
            s = i % 3
            DS(lambda e: e.dma_start(out=GS[s], in_=Gd[i]), r=[bGdq[i // 4]], w=[bGS[s]], sem=f"GS{s}")

        def p1_consume(i, tag, banks):
            s = i % 3
            for q in range(2):
                A(lambda e, q=q: e.activation(out=S[s][:, q * 512:(q + 1) * 512], in_=pb[banks[q]][:, :], func=AF.Gelu), r=[bP[banks[q]]], w=[bS[s]])
            V(lambda e: e.tensor_tensor(out=S[s][:, :], in0=S[s][:, :], in1=GS[s], op=ALU.mult), r=[bS[s], bGS[s]], w=[bS[s]])
            DS(lambda e: e.dma_start(out=hTd[i], in_=S[s][:, :]), r=[bS[s]], w=[bhTd], sem=f"S{s}")

        gemm([(down[j], j) for j in range(128)], Hv, [bH], FULL, p1_consume, extra_load=g_load)

        if stop == 7:
            for qi, jj_ in enumerate([5, 77]):
                DS(lambda e, jj_=jj_: e.dma_start(out=S[0][:, :], in_=hTd[jj_]), r=[bhTd], w=[bS[0]], sem="S0")
                V(lambda e, qi=qi: e.tensor_copy(out=X[:, qi * 1024:(qi + 1) * 1024], in_=S[0][:, :]), r=[bS[0]], w=[bX])
            DS(lambda e: e.dma_start(out=dbgo[:, 0:2048], in_=X[:, 0:2048]), r=[bX], w=[bdbg], sem="dbg")
            finish()
            return nc

        HS = [Q[:, i * 8192:(i + 1) * 8192].rearrange("p (j t) -> p j t", j=8) for i in range(3)]
        bHS = [Buf(f"HS{i}") for i in range(3)]
        kq = [0]
        NRES = 6
        Xb = X[:, :].bitcast(BF16)
        RES = [H[:, i * 8192:(i + 1) * 8192].rearrange("p (j t) -> p j t", j=8) for i in range(4)] + [Xb[:, i * 8192:(i + 1) * 8192].rearrange("p (j t) -> p j t", j=8) for i in range(2)]
        bRES = [Buf(f"RES{i}") for i in range(NRES)]
        for s_ in range(8):
            def p2_load(jg):
                k = kq[0] + jg
                sl = k % 3
                sw = k % 2
                DG(lambda e: e.dma_start(out=W[sw][:, :], in_=up[s_, jg], max_dma_last_dim=8192), w=[bW[sw]], sem=f"W{sw}")
                if jg < NRES:
                    if s_ == 0:
                        for hf in range(2):
                            DS(lambda e, hf=hf: e.dma_start(out=RES[jg][:, 4 * hf:4 * hf + 4, :], in_=hTd[jg * 8 + 4 * hf:jg * 8 + 4 * hf + 4].rearrange("j p t -> p j t")), r=[bhTd], w=[bRES[jg]] + ([bH] if jg < 4 else [bX]), sem=f"RES{jg}")
                    return
                for hf in range(2):
                    DS(lambda e, hf=hf: e.dma_start(out=HS[sl][:, 4 * hf:4 * hf + 4, :], in_=hTd[jg * 8 + 4 * hf:jg * 8 + 4 * hf + 4].rearrange("j p t -> p j t")), r=[bhTd], w=[bHS[sl], bQ], sem=f"HS{sl}")

            def p2_comp(jg):
                k = kq[0] + jg
                sl = k % 3
                sw = k % 2
                Wv = W[sw][:, :].rearrange("p (j d) -> p j d", j=8)
                hsrc = RES[jg] if jg < NRES else HS[sl]
                hbuf = bRES[jg] if jg < NRES else bHS[sl]

                def mm(e):
                    for jj in range(8):
                        for dt in range(4):
                            for th in range(2):
                                ins = e.matmul(pb[dt * 2 + th][:, :], lhsT=Wv[:, jj, dt * 128:(dt + 1) * 128], rhs=hsrc[:, jj, th * 512:(th + 1) * 512], start=(jg == 0 and jj == 0), stop=(jg == 15 and jj == 7))
                    return ins
                PE(mm, r=[bW[sw], hbuf], w=bP)
            for i in range(16 + 1):
                if i < 16:
                    p2_load(i)
                if i - 1 >= 0:
                    p2_comp(i - 1)
            kq[0] += 16
            if stop == 8:
                for k in range(4):
                    V(lambda e, k=k: e.tensor_copy(out=X[:, k * 512:(k + 1) * 512], in_=pb[k][:, :]), r=[bP[k]], w=[bX])
                V(lambda e: e.tensor_copy(out=X[:, 2048:3072], in_=HS[0][:, 0, :]), r=[bHS[0]], w=[bX])
                V(lambda e: e.tensor_copy(out=X[:, 3072:4096], in_=W[0][:, 0:1024]), r=[bW[0]], w=[bX])
                DS(lambda e: e.dma_start(out=dbgo[:, 0:4096], in_=X[:, 0:4096]), r=[bX], w=[bdbg], sem="dbg")
                finish()
                return nc
            for dt in range(4):
                di = s_ * 4 + dt
                s = di % 2
                DS(lambda e, di=di, s=s: e.dma_start(out=FS[s][:, :], in_=x1d[di]), r=[bx1d], w=[bFS[s]], sem=f"FS{s}")
                for th in range(2):
                    V(lambda e, di=di, s=s, th=th, dt=dt: e.scalar_tensor_tensor(out=FS[s][:, th * 512:(th + 1) * 512], in0=pb[dt * 2 + th][:, :], scalar=modT[:, G2 + di:G2 + di + 1], in1=FS[s][:, th * 512:(th + 1) * 512], op0=ALU.mult, op1=ALU.add), r=[bP[dt * 2 + th], bmod, bFS[s]], w=[bFS[s]])
                DS(lambda e, di=di, s=s: e.dma_start(out=outT[:, di, :], in_=FS[s][:, :]), r=[bFS[s]], w=[bout], sem=f"FSo{s}")
        finish()
    return nc


def _tile_w(w, ncol):
    N = w.shape[1]
    nt = N // ncol
    return np.ascontiguousarray(w.reshape(NCH, 128, nt, ncol).transpose(2, 1, 0, 3)).reshape(nt, 128, NCH * ncol)


def prep_shared(inp):
    f = np.float32
    sh = {}
    sh["w_ada"] = _tile_w(np.asarray(inp["w_ada"][0], f), 512)
    sh["b_ada"] = np.ascontiguousarray(np.asarray(inp["b_ada"], f).reshape(1, 24576))
    w_in = np.asarray(inp["w_in"][0], f)
    ga = np.zeros((D, 128), f)
    ga[:, :16] = w_in[:, 6144:6160]
    w_in_p = np.concatenate([w_in[:, :6144], ga, w_in[:, 6160:]], axis=1)
    sh["w_in"] = _tile_w(w_in_p, 128)
    sh["wg"] = np.ascontiguousarray(np.asarray(inp["w_gla_gate_up"][0], f))
    sh["bg"] = np.ascontiguousarray(np.asarray(inp["b_gla_gate"], f).reshape(1, 1024))
    sh["gla_gain"] = np.ascontiguousarray(np.asarray(inp["gla_out_norm"][0], f).reshape(128, 1))
    sh["qn"] = np.ascontiguousarray(np.asarray(inp["swa_q_norm"][0], f))
    sh["kn"] = np.ascontiguousarray(np.asarray(inp["swa_k_norm"][0], f))
    on = np.asarray(inp["swa_out_norm"][0], f)
    sh["on"] = np.ascontiguousarray(np.concatenate([on, on]).reshape(128, 1))
    sh["sinks"] = np.ascontiguousarray(np.asarray(inp["swa_sinks"][0], f))
    sh["w_out"] = _tile_w(np.asarray(inp["w_out"][0], f), 128)
    sh["w_q"] = _tile_w(np.asarray(inp["w_peer_q"][0], f), 128)
    k1 = np.asarray(inp["peer_sub_keys_1"][0], f)
    k2 = np.asarray(inp["peer_sub_keys_2"][0], f)
    sh["keysT"] = np.ascontiguousarray(np.stack([k1.T, k2.T], axis=1))
    dn = np.asarray(inp["peer_expert_down"][0], f)
    sh["down"] = np.ascontiguousarray(dn.reshape(128, 128, NCH, 128).transpose(1, 3, 2, 0)).reshape(128, 128, 4096)
    upw = np.asarray(inp["peer_expert_up"][0], f)
    u = upw.reshape(128, 16, 8, 8, 512).transpose(3, 1, 0, 2, 4)
    sh["up"] = np.ascontiguousarray(u).reshape(8, 16, 128, 4096)
    s_ = np.arange(128)[:, None]
    t_ = np.arange(128)[None, :]
    sh["_mcur"] = (s_ <= t_).astype(f)
    sh["_mprev"] = (s_ > t_).astype(f)
    sh["ident"] = np.eye(128, dtype=f)
    sh["iotar"] = np.ascontiguousarray(np.broadcast_to(np.arange(128, dtype=f)[None, :], (128, 128)))
    return sh


def prep_core(inp, sh, r):
    f = np.float32
    b, half = r // 2, r % 2
    x = np.asarray(inp["x"], f)
    if half == 1:
        xw = x[b]
    else:
        xw = np.concatenate([np.zeros((1024, D), f), x[b, :1024]], axis=0)
    m = {k: v for k, v in sh.items() if not k.startswith("_")}
    m["xT"] = np.ascontiguousarray(xw.reshape(2048, NCH, 128).transpose(2, 1, 0))
    m["cT"] = np.ascontiguousarray(np.asarray(inp["c"], f)[b].reshape(NCH, 128).T)
    pos = (half * 1024 - 128 + np.arange(1152)).astype(f)
    inv = (10000.0 ** (-np.arange(32, dtype=np.float64) / 32)).astype(f)
    ang = pos[:, None] * inv[None, :]
    cs = np.stack([np.cos(ang), np.sin(ang)], axis=1).astype(f)
    m["cs"] = np.ascontiguousarray(cs.reshape(9, 128, 2, 32).transpose(1, 0, 2, 3))
    fl = float(half)
    m["masks"] = np.ascontiguousarray(np.stack([sh["_mcur"], sh["_mprev"], sh["_mprev"] * fl], axis=1))
    m["flag"] = np.full((128, 1), fl, f)
    return m


_NC_CACHE = {}


def kernel(**inputs):
    sh = prep_shared(inputs)
    in_maps = [prep_core(inputs, sh, r) for r in range(N_CORES)]
    if "nc" not in _NC_CACHE:
        _NC_CACHE["nc"] = build_nc()
    nc = _NC_CACHE["nc"]
    res = run_bass_kernel_spmd(nc, in_maps, core_ids=list(range(N_CORES)))
    out = np.empty((4, 2048, D), np.float32)
    for r in range(N_CORES):
        b, half = r // 2, r % 2
        o = np.asarray(res.results[r]["outT"])
        out[b, half * 1024:(half + 1) * 1024, :] = o.transpose(2, 1, 0).reshape(1024, D)
    return out
```
